# Optimizing a Trainium2 kernel written in Bass

```python
import math
import jax, jax.numpy as jnp
from jax import lax
import numpy as np

D_MODEL = 2048
BATCH = 2
SEQ = 4096
DEPTH = 4

N_A_LAYERS = DEPTH // 2
N_B_LAYERS = DEPTH - N_A_LAYERS
GM_CHUNK = 128
GM_WIDTH = D_MODEL
GM_GROUPS = 16
GM_GROUP_DIM = GM_WIDTH // GM_GROUPS
N_HEADS = 16
HEAD_DIM = D_MODEL // N_HEADS
MOBA_BLOCK = 256
MOBA_TOPK = 3
MOBA_Q_CHUNK = 16
N_EXPERTS = 32
TOP_K_EXPERTS = 4
D_EXPERT = D_MODEL // 4
SWIGLU_LIMIT = 7.0
SWIGLU_ALPHA = 1.702
MOE_TOKEN_BLOCK = 128
DEEPNORM_ALPHA = (2.0 * DEPTH) ** 0.25
DEEPNORM_BETA = (8.0 * DEPTH) ** -0.25
LN_EPS = 1e-5

kernel_name = "yoco_gmlp_moba_moe_deepnorm_adaln"


def _layer_norm(x, g, b):
    xf = x.astype(jnp.float32)
    mu = jnp.mean(xf, axis=-1, keepdims=True)
    var = jnp.mean(jnp.square(xf - mu), axis=-1, keepdims=True)
    y = (xf - mu) * lax.rsqrt(var + LN_EPS) * g.astype(jnp.float32) + b.astype(jnp.float32)
    return y.astype(x.dtype)


def _modulate(x, shift, scale):
    return x * (1.0 + scale[:, None, :]) + shift[:, None, :]


def _gmlp_chunk_mixer(h, w_in, b_in, lnv_g, lnv_b, w_s, b_s, w_out):
    B, S, _ = h.shape
    z = jax.nn.gelu(h @ w_in + b_in)
    u, v = z[..., :GM_WIDTH], z[..., GM_WIDTH:]
    v = _layer_norm(v, lnv_g, lnv_b)
    v = v.reshape(B, S // GM_CHUNK, GM_CHUNK, GM_GROUPS, GM_GROUP_DIM)
    causal = jnp.tril(jnp.ones((GM_CHUNK, GM_CHUNK), dtype=w_s.dtype))
    ws = w_s * causal[None]
    sv = jnp.einsum('gts,bnsgc->bntgc', ws, v) + b_s.T[:, :, None]
    y = u * sv.reshape(B, S, GM_WIDTH)
    return y @ w_out


def _shared_kv(x, c_act, kv_ada_w, kv_ada_b, w_kv):
    B, S, _ = x.shape
    shift, scale = jnp.split(c_act @ kv_ada_w + kv_ada_b, 2, axis=-1)
    kv = _modulate(x, shift, scale) @ w_kv
    kv = kv.reshape(B, S, 2, N_HEADS, HEAD_DIM)
    nb = -(-S // MOBA_BLOCK)
    pad = nb * MOBA_BLOCK - S
    kv = jnp.pad(kv, ((0, 0), (0, pad), (0, 0), (0, 0), (0, 0)))
    kv = kv.reshape(B, nb, MOBA_BLOCK, 2, N_HEADS, HEAD_DIM).transpose(3, 0, 4, 1, 2, 5)
    k_blocks, v_blocks = kv[0], kv[1]
    k_means = jnp.mean(k_blocks.astype(jnp.float32), axis=3).astype(k_blocks.dtype)
    return k_blocks, v_blocks, k_means


def _moba_attention(h, w_q, w_out, k_blocks, v_blocks, k_means):
    B, S, _ = h.shape
    nb = k_blocks.shape[2]
    topk = min(MOBA_TOPK, nb)
    scale = HEAD_DIM ** -0.5
    q = (h @ w_q).reshape(B, S, N_HEADS, HEAD_DIM).transpose(0, 2, 1, 3)
    k_flat = k_blocks.reshape(B * N_HEADS * nb, MOBA_BLOCK, HEAD_DIM)
    v_flat = v_blocks.reshape(B * N_HEADS * nb, MOBA_BLOCK, HEAD_DIM)
    bh = (jnp.arange(B, dtype=jnp.int32)[:, None] * N_HEADS
          + jnp.arange(N_HEADS, dtype=jnp.int32)[None, :])[:, :, None, None]
    blk_ids = jnp.arange(nb, dtype=jnp.int32)
    n_chunks = S // MOBA_Q_CHUNK

    def one_chunk(i):
        start = i * MOBA_Q_CHUNK
        own = start // MOBA_BLOCK
        qc = lax.dynamic_slice_in_dim(q, start, MOBA_Q_CHUNK, axis=2)
        gs = jnp.einsum('bhqd,bhnd->bhqn', qc, k_means, preferred_element_type=jnp.float32)
        gs = jnp.where(blk_ids < own, gs, -jnp.inf)
        _, idx = lax.top_k(gs, topk)
        valid = idx < own
        flat = bh * nb + idx
        kg = jnp.take(k_flat, flat, axis=0)
        vg = jnp.take(v_flat, flat, axis=0)
        s_sel = jnp.einsum('bhqd,bhqkjd->bhqkj', qc, kg,
                           preferred_element_type=jnp.float32) * scale
        s_sel = jnp.where(valid[..., None], s_sel, -jnp.inf)
        s_sel = s_sel.reshape(B, N_HEADS, MOBA_Q_CHUNK, topk * MOBA_BLOCK)
        k_own = lax.dynamic_index_in_dim(k_blocks, own, axis=2, keepdims=False)
        v_own = lax.dynamic_index_in_dim(v_blocks, own, axis=2, keepdims=False)
        s_own = jnp.einsum('bhqd,bhjd->bhqj', qc, k_own,
                           preferred_element_type=jnp.float32) * scale
        qpos = start + jnp.arange(MOBA_Q_CHUNK, dtype=jnp.int32)
        kpos = own * MOBA_BLOCK + jnp.arange(MOBA_BLOCK, dtype=jnp.int32)
        s_own = jnp.where(kpos[None, :] <= qpos[:, None], s_own, -jnp.inf)
        p = jax.nn.softmax(jnp.concatenate([s_sel, s_own], axis=-1), axis=-1)
        p_sel = p[..., :topk * MOBA_BLOCK].reshape(
            B, N_HEADS, MOBA_Q_CHUNK, topk, MOBA_BLOCK).astype(vg.dtype)
        p_own = p[..., topk * MOBA_BLOCK:].astype(v_own.dtype)
        out = (jnp.einsum('bhqkj,bhqkjd->bhqd', p_sel, vg)
               + jnp.einsum('bhqj,bhjd->bhqd', p_own, v_own))
        return out.astype(h.dtype)

    o = lax.map(one_chunk, jnp.arange(n_chunks, dtype=jnp.int32))
    o = o.transpose(1, 0, 3, 2, 4).reshape(B, S, N_HEADS * HEAD_DIM)
    return o @ w_out


def _routed_moe(h, w_r, b_r, w_gu, b_gu, w_down, b_down):
    B, S, D = h.shape
    t = h.reshape(B * S, D)
    logits = (t @ w_r).astype(jnp.float32) + b_r.astype(jnp.float32)
    top_v, top_i = lax.top_k(logits, TOP_K_EXPERTS)
    top_w = jax.nn.softmax(top_v, axis=-1)
    gates = jnp.sum(jax.nn.one_hot(top_i, N_EXPERTS, dtype=jnp.float32) * top_w[..., None], axis=1)
    gates = gates.astype(h.dtype)
    nblk = (B * S) // MOE_TOKEN_BLOCK

    def block(args):
        tb, gb = args
        gu = jnp.einsum('td,edf->tef', tb, w_gu) + b_gu
        g = jnp.minimum(gu[..., :D_EXPERT], SWIGLU_LIMIT)
        u = jnp.clip(gu[..., D_EXPERT:], -SWIGLU_LIMIT, SWIGLU_LIMIT)
        act = (u + 1.0) * g * jax.nn.sigmoid(SWIGLU_ALPHA * g)
        return jnp.einsum('tef,efd->td', act * gb[..., None], w_down) + gb @ b_down

    out = lax.map(block, (t.reshape(nblk, MOE_TOKEN_BLOCK, D),
                          gates.reshape(nblk, MOE_TOKEN_BLOCK, N_EXPERTS)))
    return out.reshape(B, S, D)


def _normal(key, shape, scale):
    return jax.random.normal(key, shape, jnp.float32) * scale


def setup_inputs(seed: int = 0) -> dict:
    key = jax.random.key(seed)
    ks = jax.random.split(key, 26)
    D, HD, W2 = D_MODEL, N_HEADS * HEAD_DIM, 2 * GM_WIDTH
    beta = DEEPNORM_BETA
    x = _normal(ks[0], (BATCH, SEQ, D), 1.0)
    c = _normal(ks[1], (BATCH, D), 1.0)
    ada_w = _normal(ks[2], (DEPTH, D, 6 * D), 0.1 * D ** -0.5)
    ada_b = _normal(ks[3], (DEPTH, 6 * D), 0.01)
    ln_g = 1.0 + _normal(ks[4], (DEPTH, 2, D), 0.01)
    ln_b = _normal(ks[5], (DEPTH, 2, D), 0.01)
    gm_w_in = _normal(ks[6], (N_A_LAYERS, D, W2), D ** -0.5)
    gm_b_in = _normal(ks[7], (N_A_LAYERS, W2), 0.01)
    gm_lnv_g = 1.0 + _normal(ks[8], (N_A_LAYERS, GM_WIDTH), 0.01)
    gm_lnv_b = _normal(ks[9], (N_A_LAYERS, GM_WIDTH), 0.01)
    gm_w_s = _normal(ks[10], (N_A_LAYERS, GM_GROUPS, GM_CHUNK, GM_CHUNK), 0.5 * GM_CHUNK ** -0.5)
    gm_b_s = 1.0 + _normal(ks[11], (N_A_LAYERS, GM_GROUPS, GM_CHUNK), 0.01)
    gm_w_out = _normal(ks[12], (N_A_LAYERS, GM_WIDTH, D), beta * GM_WIDTH ** -0.5)
    kv_ada_w = _normal(ks[13], (D, 2 * D), 0.1 * D ** -0.5)
    kv_ada_b = _normal(ks[14], (2 * D,), 0.01)
    w_kv = jnp.concatenate([_normal(ks[15], (D, HD), D ** -0.5),
                            _normal(ks[16], (D, HD), beta * D ** -0.5)], axis=1)
    attn_w_q = _normal(ks[17], (N_B_LAYERS, D, HD), D ** -0.5)
    attn_w_out = _normal(ks[18], (N_B_LAYERS, HD, D), beta * HD ** -0.5)
    moe_w_router = _normal(ks[19], (DEPTH, D, N_EXPERTS), D ** -0.5)
    moe_b_router = _normal(ks[20], (DEPTH, N_EXPERTS), 0.01)
    moe_w_gu = _normal(ks[21], (DEPTH, N_EXPERTS, D, 2 * D_EXPERT), D ** -0.5)
    moe_b_gu = _normal(ks[22], (DEPTH, N_EXPERTS, 2 * D_EXPERT), 0.01)
    moe_w_down = _normal(ks[23], (DEPTH, N_EXPERTS, D_EXPERT, D), beta * D_EXPERT ** -0.5)
    moe_b_down = _normal(ks[24], (DEPTH, N_EXPERTS, D), 0.01)
    return {"x": x, "c": c, "ada_w": ada_w, "ada_b": ada_b, "ln_g": ln_g, "ln_b": ln_b,
            "gm_w_in": gm_w_in, "gm_b_in": gm_b_in, "gm_lnv_g": gm_lnv_g, "gm_lnv_b": gm_lnv_b,
            "gm_w_s": gm_w_s, "gm_b_s": gm_b_s, "gm_w_out": gm_w_out,
            "kv_ada_w": kv_ada_w, "kv_ada_b": kv_ada_b, "w_kv": w_kv,
            "attn_w_q": attn_w_q, "attn_w_out": attn_w_out,
            "moe_w_router": moe_w_router, "moe_b_router": moe_b_router,
            "moe_w_gu": moe_w_gu, "moe_b_gu": moe_b_gu,
            "moe_w_down": moe_w_down, "moe_b_down": moe_b_down}


def reference(x, c, ada_w, ada_b, ln_g, ln_b, gm_w_in, gm_b_in, gm_lnv_g, gm_lnv_b,
              gm_w_s, gm_b_s, gm_w_out, kv_ada_w, kv_ada_b, w_kv, attn_w_q, attn_w_out,
              moe_w_router, moe_b_router, moe_w_gu, moe_b_gu, moe_w_down, moe_b_down):
    c_act = jax.nn.silu(c)
    k_blocks = v_blocks = k_means = None
    for l in range(DEPTH):
        mods = c_act @ ada_w[l] + ada_b[l]
        sh1, sc1, g1, sh2, sc2, g2 = jnp.split(mods, 6, axis=-1)
        h = _modulate(x, sh1, sc1)
        if l < N_A_LAYERS:
            i = l
            h = _gmlp_chunk_mixer(h, gm_w_in[i], gm_b_in[i], gm_lnv_g[i], gm_lnv_b[i],
                                  gm_w_s[i], gm_b_s[i], gm_w_out[i])
        else:
            j = l - N_A_LAYERS
            h = _moba_attention(h, attn_w_q[j], attn_w_out[j], k_blocks, v_blocks, k_means)
        x = _layer_norm(DEEPNORM_ALPHA * x + (1.0 + g1)[:, None, :] * h, ln_g[l, 0], ln_b[l, 0])
        h = _routed_moe(_modulate(x, sh2, sc2), moe_w_router[l], moe_b_router[l],
                        moe_w_gu[l], moe_b_gu[l], moe_w_down[l], moe_b_down[l])
        x = _layer_norm(DEEPNORM_ALPHA * x + (1.0 + g2)[:, None, :] * h, ln_g[l, 1], ln_b[l, 1])
        if l == N_A_LAYERS - 1:
            k_blocks, v_blocks, k_means = _shared_kv(x, c_act, kv_ada_w, kv_ada_b, w_kv)
    return x
```

```python
import math
import numpy as np
import ml_dtypes
import concourse.bass as bass
import concourse.mybir as mybir
from concourse.bass_utils import run_bass_kernel_spmd

F32 = mybir.dt.float32
BF16 = mybir.dt.bfloat16
AF = mybir.ActivationFunctionType
ALU = mybir.AluOpType
AX = mybir.AxisListType

D = 2048
KC = 16
NT = 8
TOK = 1024
NE = 32
ALPHA = 8.0 ** 0.25
EPS = 1e-5
SW_A = 1.702
C7 = SW_A * 7.0 / (1.0 + math.exp(-SW_A * 7.0))
NEG = -30000.0
SEM_CAP = 30000
NSLOT = 3


class Eng:
    def __init__(self, fw, name, handle):
        self.fw, self.name, self.h = fw, name, handle
        self.nsem, self.cnt, self.waited = 0, 0, {}
        self.pend_r, self.pend_w = [], []
        self._newsem()

    def _newsem(self):
        self.sem = self.fw.nc.alloc_semaphore(f"{self.name}_e{self.nsem}")
        self.nsem += 1
        self.cnt = 0

    def wait(self, tok):
        sem, val = tok
        if self.waited.get(sem.name, 0) >= val:
            return
        self.h.wait_ge(sem, val)
        self.waited[sem.name] = val

    def mark(self, ins):
        if self.cnt >= SEM_CAP:
            self._newsem()
        ins.then_inc(self.sem, 1)
        self.cnt += 1
        return (self.sem, self.cnt)


class FW:
    def __init__(self, nc):
        self.nc = nc
        self.dry = False
        self.E = {"pe": Eng(self, "pe", nc.tensor), "act": Eng(self, "act", nc.scalar),
                  "dve": Eng(self, "dve", nc.vector), "pool": Eng(self, "pool", nc.gpsimd),
                  "sp": Eng(self, "sp", nc.sync)}
        self.lw, self.rd, self.dsem = {}, {}, {}
        self.ninst = 0

    @staticmethod
    def _px(R, W):
        Rp = [k for k in R if isinstance(k, tuple) and k[0] == "ps"]
        if Rp:
            R = [k for k in R if not (isinstance(k, tuple) and k[0] == "ps")]
            W = list(W) + Rp
        return list(R), list(W)

    def _deps(self, eng, R, W):
        e = self.E[eng]
        pe = self.E["pe"]
        if eng != "pe" and pe.pend_r:
            for k in W:
                if k in pe.pend_r:
                    raise RuntimeError(f"write to {k} on {eng} while PE read has no token yet")
        for k in R:
            t = self.lw.get(k)
            if t is not None and not (t[1] == "pe" and eng == "pe"):
                e.wait(t[0])
        for k in W:
            t = self.lw.get(k)
            if t is not None and not (t[1] == "pe" and eng == "pe") and t[1] != getattr(self, "_skip_src", None):
                e.wait(t[0])
            for t in self.rd.get(k, ()):
                if not (t[1] == "pe" and eng == "pe"):
                    e.wait(t[0])

    def _commit(self, src, tok, R, W):
        for k in W:
            self.lw[k] = (tok, src)
            self.rd[k] = []
        for k in R:
            lst = self.rd.setdefault(k, [])
            lst[:] = [x for x in lst if x[1] != src]
            lst.append((tok, src))

    def _pe_done(self, ins, R, W):
        e = self.E["pe"]
        tok = e.mark(ins)
        R = list(R) + e.pend_r
        W = list(W) + e.pend_w
        e.pend_r, e.pend_w = [], []
        self._commit("pe", tok, R, W)
        return tok

    def op(self, eng, fn, R=(), W=()):
        if self.dry:
            return None
        R, W = self._px(R, W)
        self._deps(eng, R, W)
        ins = fn()
        self.ninst += 1
        tok = self.E[eng].mark(ins)
        self._commit(eng, tok, R, W)
        return tok

    def mm(self, out, pairs, R=(), W=()):
        if self.dry:
            return None
        self._deps("pe", R, W)
        n = len(pairs)
        ins = None
        for i, (a, b) in enumerate(pairs):
            ins = self.nc.tensor.matmul(out, a, b, start=(i == 0), stop=(i == n - 1))
        self.ninst += n
        return self._pe_done(ins, R, W)

    def mm1(self, out, a, b, start, stop, R=(), W=(), last=False):
        if self.dry:
            return None
        self._deps("pe", R, W)
        ins = self.nc.tensor.matmul(out, a, b, start=start, stop=stop)
        self.ninst += 1
        if last:
            return self._pe_done(ins, R, W)
        e = self.E["pe"]
        e.pend_r += list(R)
        e.pend_w += list(W)
        return None

    def tr(self, out, in_, ident, R=(), W=(), last=True):
        if self.dry:
            return None
        self._deps("pe", R, W)
        ins = self.nc.tensor.transpose(out, in_, ident)
        self.ninst += 1
        if last:
            return self._pe_done(ins, R, W)
        e = self.E["pe"]
        e.pend_r += list(R)
        e.pend_w += list(W)
        return None

    def dma(self, q, out, in_, chan, R=(), W=()):
        if self.dry:
            return None
        self._skip_src = "dma:" + chan
        self._deps(q, R, W)
        self._skip_src = None
        if chan not in self.dsem:
            self.dsem[chan] = [self.nc.alloc_semaphore(f"d_{chan}"), 0]
        ds = self.dsem[chan]
        self.E[q].h.dma_start(out=out, in_=in_).then_inc(ds[0], 16)
        self.ninst += 1
        ds[1] += 16
        tok = (ds[0], ds[1])
        self._commit("dma:" + chan, tok, R, W)
        return tok

    def barrier(self):
        if self.dry:
            return
        best = {}
        def add(tok):
            sem, val = tok
            if best.get(sem.name, (None, 0))[1] < val:
                best[sem.name] = (sem, val)
        for (t, s) in self.lw.values():
            add(t)
        for lst in self.rd.values():
            for (t, s) in lst:
                add(t)
        for en in ("pe", "act", "dve", "pool", "sp"):
            for tok in best.values():
                self.E[en].wait(tok)

    def finish(self, toks):
        for t in toks:
            if t is not None:
                self.E["sp"].wait(t)


class Ring:
    def __init__(self, fw, wr):
        self.fw, self.wr = fw, wr
        self.plan, self.cons, self.issued = [], 0, 0

    def reset(self):
        self.cons, self.issued = 0, 0

    def next(self, tag, loader):
        if self.fw.dry:
            self.plan.append((tag, loader))
            i = len(self.plan) - 1
            return self.wr[:, i % NSLOT, :], ("WR", i % NSLOT)
        i = self.cons
        assert self.plan[i][0] == tag, (self.plan[i][0], tag)
        while self.issued < min(len(self.plan), i + NSLOT - 1):
            k = self.issued
            s = k % NSLOT
            self.plan[k][1](self.wr[:, s, :], s)
            self.issued += 1
        self.cons += 1
        return self.wr[:, i % NSLOT, :], ("WR", i % NSLOT)


class Arena:
    def __init__(self, t, n):
        self.t, self.n, self.off = t, n, 0

    def f32(self, n):
        assert self.off + n <= self.n, ("arena overflow", self.off, n, self.n)
        v = self.t[:, self.off:self.off + n]
        self.off += n
        return v

    def bf16(self, n):
        m = (n + 1) // 2
        return self.f32(m).bitcast(BF16)[:, 0:n]


class Prog:
    def __init__(self, stage, stop_after=None, npass=1):
        self.npass = npass
        self.stage = stage
        self.layers = [0, 1] if stage == "A" else [2, 3]
        self.stop_after = stop_after
        nc = self.nc = bass.Bass("TRN2", target_bir_lowering=False)
        self.fw = FW(nc)
        dt = lambda name, shape, ty=F32, kind="ExternalInput": nc.dram_tensor(name, list(shape), ty, kind=kind).ap()
        self.io = [dict() for _ in range(npass)]
        for p in range(npass):
            self.io[p]["x_d"] = dt(f"x{p}", [TOK, D])
            self.io[p]["cl_d"] = dt(f"cl{p}", [128, KC])
        self.ada_w = dt("ada_w", [2, D, 6 * D])
        self.ada_b = dt("ada_b", [2, 6 * D])
        self.ada_bT = dt("ada_bT", [2, 128, 96])
        self.ln_g = dt("ln_g", [2, 2, D])
        self.ln_b = dt("ln_b", [2, 2, D])
        self.w_r = dt("w_r", [2, D, NE])
        self.b_rT = dt("b_rT", [2, NE, 1])
        self.w_gu = dt("w_gu", [2, NE, D, 1024])
        self.b_guT = dt("b_guT", [2, 128, NE, 8])
        self.w_dn = dt("w_dn", [2, NE, 512, D])
        self.b_dn = dt("b_dn", [2, NE, D])
        if stage == "A":
            self.w_in = dt("w_in", [2, D, 2 * D])
            self.b_in = dt("b_in", [2, 2 * D])
            self.b_inuT = dt("b_inuT", [2, 128, KC])
            self.lnv_gT = dt("lnv_gT", [2, 128, KC])
            self.lnv_bT = dt("lnv_bT", [2, 128, KC])
            self.w_s = dt("w_s", [2, 16, 128, 128])
            self.b_s = dt("b_s", [2, 16 * 128])
            self.w_o = dt("w_o", [2, D, D])
            self.kv_ada_w = dt("kv_ada_w", [D, 2 * D])
            self.kv_ada_bT = dt("kv_ada_bT", [128, 32])
            self.w_kv = dt("w_kv", [D, 2 * D])
            for p in range(npass):
                self.io[p]["kt_o"] = dt(f"kt_o{p}", [16, 128, TOK], BF16, "ExternalOutput")
                self.io[p]["v_o"] = dt(f"v_o{p}", [TOK, D], BF16, "ExternalOutput")
                self.io[p]["km_o"] = dt(f"km_o{p}", [128, 16, 4], F32, "ExternalOutput")
        else:
            self.w_q = dt("w_q", [2, D, D])
            self.w_ao = dt("w_ao", [2, D, D])
            for p in range(npass):
                self.io[p]["kt_all"] = dt(f"kt_all{p}", [16, 128, 4096], BF16)
                self.io[p]["v_all"] = dt(f"v_all{p}", [16, 128, 32, 128], BF16)
                self.io[p]["kt_own"] = dt(f"kt_own{p}", [16, 128, TOK], BF16)
                self.io[p]["v_own"] = dt(f"v_own{p}", [16, 128, NT, 128], BF16)
                self.io[p]["km_all"] = dt(f"km_all{p}", [128, 16, 16])
                self.io[p]["pastm"] = dt(f"pastm{p}", [128, NT, 16])
        for p in range(npass):
            self.io[p]["y_d"] = dt(f"y{p}", [TOK, D], F32, "ExternalOutput")

        sb = nc.alloc_sbuf_tensor
        self.X = sb("X", [128, NT, D], F32)
        self.WR = sb("WR", [128, NSLOT, 8192], BF16)
        self.A0 = sb("A0", [128, 16384], BF16)
        self.ident = sb("ident", [128, 128], F32)
        self.ones_bf = sb("ones_bf", [128, 128], BF16)
        self.cact = sb("cact", [128, KC], F32)
        self.cact_bf = sb("cact_bf", [128, KC], BF16)
        self.cact_rep = sb("cact_rep", [128, KC, 128], BF16)
        self.modP = sb("modP", [128, 4, KC], F32)
        self.adabT = sb("adabT", [128, 96], F32)
        self.small = sb("small", [128, 64], F32)
        ARN = 14700
        self.arena = Arena(sb("ARENA", [128, ARN], F32), ARN)
        self.ps = [nc.alloc_psum_tensor(f"ps{i}", [128, 512], F32) for i in range(8)]
        self.ring = Ring(self.fw, self.WR)
        self.out_toks = []

    def PK(self, i):
        return ("ps", i)

    def HT(self):
        return self.A0[:, :].rearrange("p (k t) -> p k t", k=KC)

    def set_pass(self, p):
        for k, v in self.io[p].items():
            setattr(self, k, v)

    def init_consts(self, first=True):
        nc, fw = self.nc, self.fw
        if first:
            fw.op("pool", lambda: nc.gpsimd.memset(self.ident[:], 1.0), W=["ident"])
            fw.op("pool", lambda: nc.gpsimd.affine_select(self.ident[:], self.ident[:], pattern=[[-1, 128]],
                                                          compare_op=ALU.is_equal, fill=0.0, base=0,
                                                          channel_multiplier=1), R=["ident"], W=["ident"])
            fw.op("pool", lambda: nc.gpsimd.memset(self.ones_bf[:], 1.0), W=["ones"])
        fw.dma("sp", self.cact[:], self.cl_d, "c", W=["cact"])
        fw.op("act", lambda: nc.scalar.activation(self.cact[:], self.cact[:], AF.Silu), R=["cact"], W=["cact"])
        fw.op("dve", lambda: nc.vector.tensor_copy(self.cact_bf[:], self.cact[:]), R=["cact"], W=["cactbf"])
        for kc in range(KC):
            fw.op("dve", lambda kc=kc: nc.vector.tensor_scalar(self.cact_rep[:, kc, :], self.ones_bf[:],
                                                               self.cact[:, kc:kc + 1], None, op0=ALU.mult),
                  R=["cact", "ones"], W=["cactrep"])
        for t in range(NT):
            fw.dma("sp", self.X[:, t, :], self.x_d[t * 128:(t + 1) * 128, :], f"x{t}", W=[("X", t)])

    def ada_piece_loader(self, w3, col0, chan_tag):
        src = w3.rearrange("(k p) f -> p k f", p=128)[:, :, col0:col0 + 512]
        def loader(slot, s):
            dst = slot.rearrange("p (k f) -> p k f", k=KC)
            self.fw.dma("pool", dst, src, f"wr{s}", W=[("WR", s)])
        return loader

    def modsP(self, wmat, vecs, bT, dst_cols):
        nc, fw = self.nc, self.fw
        for (vi, add1), dst in zip(vecs, dst_cols):
            bank = self.ps[6]
            for n in range(4):
                slot, wk = self.ring.next(("adaP", vi, n), self.ada_piece_loader(wmat, vi * D + n * 512, "a"))
                w = slot.rearrange("p (k f) -> p k f", k=KC)
                for c in range(4):
                    col = n * 4 + c
                    for kc in range(KC):
                        fw.mm1(bank[:, col:col + 1], w[:, kc, c * 128:(c + 1) * 128], self.cact_bf[:, kc:kc + 1],
                               start=(kc == 0), stop=(kc == KC - 1), R=[wk, "cactbf"], W=[self.PK(6)],
                               last=(kc == KC - 1 and c == 3))
            fw.op("dve", lambda dst=dst, vi=vi: nc.vector.tensor_tensor(dst, bank[:, 0:KC], bT[:, vi * KC:(vi + 1) * KC], op=ALU.add),
                  R=[self.PK(6), "adabT"], W=["modP"])
            if add1:
                fw.op("dve", lambda dst=dst: nc.vector.tensor_scalar(dst, dst, 1.0, None, op0=ALU.add), R=["modP"], W=["modP"])

    def modsBC(self, l, vi, dst, tmp2):
        nc, fw = self.nc, self.fw
        for n in range(4):
            slot, wk = self.ring.next(("adaB", l, vi, n), self.ada_piece_loader(self.ada_w[l], vi * D + n * 512, "a"))
            w = slot.rearrange("p (k f) -> p k f", k=KC)
            tb = tmp2[n % 2]
            fw.dma("sp", tb, self.ada_b[l, vi * D + n * 512: vi * D + (n + 1) * 512].partition_broadcast(128),
                   f"adab{n % 2}", W=[("adab", n % 2)])
            bk = 4 + (n % 2)
            fw.mm(self.ps[bk][:, :], [(self.cact_rep[:, kc, :], w[:, kc, :]) for kc in range(KC)],
                  R=[wk, "cactrep"], W=[self.PK(bk)])
            fw.op("dve", lambda n=n, tb=tb, bk=bk: nc.vector.scalar_tensor_tensor(dst[:, n * 512:(n + 1) * 512], self.ps[bk][:, :], 1.0, tb,
                                                                                op0=ALU.add, op1=ALU.add),
                  R=[self.PK(bk), ("adab", n % 2)], W=[("gbc", n)])

    def build_ht(self, tiles, sc, bi, dst, ntg, router=None):
        nc, fw = self.nc, self.fw
        k = 0
        for kc in range(KC):
            for tg in range(ntg):
                bk = 6 + (k % 2)
                k += 1
                bank = self.ps[bk]
                for i in range(4):
                    t = tiles[tg * 4 + i]
                    fw.tr(bank[:, i * 128:(i + 1) * 128], self.X[:, t, kc * 128:(kc + 1) * 128], self.ident[:],
                          R=[("X", t), "ident"], W=[self.PK(bk)], last=(i == 3))
                fw.op("act", lambda kc=kc, tg=tg, bank=bank: nc.scalar.activation(
                    dst[:, kc, tg * 512:(tg + 1) * 512], bank[:, :], AF.Identity,
                    bias=bi[:, kc:kc + 1], scale=sc[:, kc:kc + 1]),
                    R=[self.PK(bk), "modP"], W=[("HT", kc, tg)])
                if router is not None:
                    hf, wr32, lt = router
                    hb = hf[(kc * ntg + tg) % 2]
                    hk = ("hf", (kc * ntg + tg) % 2)
                    fw.op("dve", lambda kc=kc, bank=bank, hb=hb: nc.vector.tensor_scalar(
                        hb, bank[:, :], sc[:, kc:kc + 1], bi[:, kc:kc + 1], op0=ALU.mult, op1=ALU.add),
                        R=[self.PK(bk), "modP"], W=[hk])
                    fw.mm(self.ps[4 + tg][:, :], [(wr32[:, kc, :], hb)], R=[hk, "wr32"], W=[self.PK(4 + tg)])
                    lts = lt[0:NE, tg * 512:(tg + 1) * 512]
                    if kc == 0:
                        fw.op("dve", lambda tg=tg, lts=lts: nc.vector.tensor_copy(lts, self.ps[4 + tg][0:NE, :]), R=[self.PK(4 + tg)], W=[("LTs", tg)])
                    else:
                        fw.op("dve", lambda tg=tg, lts=lts: nc.vector.tensor_tensor(lts, lts, self.ps[4 + tg][0:NE, :], op=ALU.add),
                              R=[self.PK(4 + tg), ("LTs", tg)], W=[("LTs", tg)])

    def layer_norm(self, tiles, g_ap, b_ap, gbc, bbc, st, mv):
        nc, fw = self.nc, self.fw
        fw.dma("sp", gbc, g_ap.partition_broadcast(128), "lng", W=["lng"])
        fw.dma("sp", bbc, b_ap.partition_broadcast(128), "lnb", W=["lnb"])
        for t in tiles:
            xt = self.X[:, t, :]
            xk = ("X", t)
            for c in range(4):
                fw.op("dve", lambda c=c, xt=xt: nc.vector.bn_stats(st[:, c, :], xt[:, c * 512:(c + 1) * 512]), R=[xk], W=[("st", c)])
            fw.op("dve", lambda: nc.vector.bn_aggr(mv[:, 0:2], st[:, :, :]), R=[("st", c) for c in range(4)], W=["mv"])
            fw.op("dve", lambda: nc.vector.tensor_scalar(mv[:, 2:3], mv[:, 1:2], EPS, None, op0=ALU.add), R=["mv"], W=["mv2"])
            fw.op("act", lambda: nc.scalar.activation(mv[:, 2:3], mv[:, 2:3], AF.Sqrt), R=["mv2"], W=["mv2"])
            fw.op("dve", lambda: nc.vector.reciprocal(mv[:, 2:3], mv[:, 2:3]), R=["mv2"], W=["mv2"])
            fw.op("dve", lambda: nc.vector.scalar_tensor_tensor(mv[:, 3:4], mv[:, 0:1], -1.0, mv[:, 2:3], op0=ALU.mult, op1=ALU.mult),
                  R=["mv", "mv2"], W=["mv3"])
            fw.op("act", lambda xt=xt: nc.scalar.activation(xt, xt, AF.Identity, bias=mv[:, 3:4], scale=mv[:, 2:3]),
                  R=[xk, "mv2", "mv3"], W=[xk])
            fw.op("dve", lambda xt=xt: nc.vector.tensor_tensor(xt, xt, gbc, op=ALU.mult), R=[xk, "lng"], W=[xk])
            fw.op("dve", lambda xt=xt: nc.vector.tensor_tensor(xt, xt, bbc, op=ALU.add), R=[xk, "lnb"], W=[xk])

    def resid_evac(self, bk, t, n, gbc, tmp, tk):
        nc, fw = self.nc, self.fw
        xs = self.X[:, t, n * 512:(n + 1) * 512]
        fw.op("dve", lambda: nc.vector.tensor_tensor(tmp, self.ps[bk][:, :], gbc[:, n * 512:(n + 1) * 512], op=ALU.mult),
              R=[self.PK(bk), ("gbc", n)], W=[tk])
        fw.op("dve", lambda: nc.vector.scalar_tensor_tensor(xs, xs, ALPHA, tmp, op0=ALU.mult, op1=ALU.add),
              R=[("X", t), tk], W=[("X", t)])

    def gmlp(self, li):
        nc, fw, ar = self.nc, self.fw, self.arena
        fw.barrier()
        ar.off = 0
        st = ar.f32(24).rearrange("p (c s) -> p c s", c=4)
        mv = ar.f32(4)
        mark = ar.off
        VB = ar.bf16(4 * D).rearrange("p (t f) -> p t f", t=4)
        g1bc = ar.f32(D)
        Bt = ar.f32(D).rearrange("p (g t) -> p g t", g=16)
        WST = ar.bf16(D).rearrange("p (g t) -> p g t", g=16)
        binv = ar.bf16(D)
        adab = [ar.f32(512), ar.f32(512)]
        tmp = [ar.f32(512), ar.f32(512)]
        binu = ar.f32(KC)
        lnvg = ar.f32(KC)
        lnvb = ar.f32(KC)
        a0f = self.A0[:, :].bitcast(F32)
        wss = a0f[:, 0:2048].rearrange("p (g s) -> p g s", g=16)
        bsbc = a0f[:, 2048:4096].rearrange("p (g t) -> p g t", g=16)
        fw.dma("sp", wss, self.w_s[li].rearrange("g t s -> t g s"), "wss", W=["wss"])
        fw.dma("sp", bsbc, self.b_s[li].partition_broadcast(128), "bsbc", W=["bsbc"])
        fw.dma("pool", binv[0:1, :], self.b_in[li:li + 1, D:2 * D], "binv", W=["binv"])
        fw.dma("sp", binu, self.b_inuT[li], "binu", W=["binu"])
        fw.dma("sp", lnvg, self.lnv_gT[li], "lnvg", W=["lnvg"])
        fw.dma("sp", lnvb, self.lnv_bT[li], "lnvb", W=["lnvb"])
        fw.dma("sp", self.adabT[:], self.ada_bT[li], "adabT", W=["adabT"])
        fw.op("pool", lambda: nc.gpsimd.affine_select(wss, wss, pattern=[[0, 16], [-1, 128]], compare_op=ALU.is_ge,
                                                      fill=0.0, base=0, channel_multiplier=1), R=["wss"], W=["wss"])
        for gq in range(4):
            bk = 6 + gq % 2
            for i in range(4):
                g = gq * 4 + i
                fw.tr(self.ps[bk][:, i * 128:(i + 1) * 128], wss[:, g, :], self.ident[:], R=["wss", "ident"], W=[self.PK(bk)], last=(i == 3))
            fw.op("act", lambda gq=gq, bk=bk: nc.scalar.copy(WST[:, gq * 4:(gq + 1) * 4, :], self.ps[bk][:, :].rearrange("p (g t) -> p g t", g=4)),
                  R=[self.PK(bk)], W=[("WST", gq)])
        for gq in range(4):
            bk = 6 + gq % 2
            fw.mm(self.ps[bk][:, :], [(self.ones_bf[:], WST[:, gq * 4:(gq + 1) * 4, :])], R=[("WST", gq), "ones"], W=[self.PK(bk)])
            for i in range(4):
                g = gq * 4 + i
                fw.op("dve", lambda g=g, i=i, bk=bk: nc.vector.scalar_tensor_tensor(
                    Bt[:, g, :], self.ps[bk][:, i * 128:(i + 1) * 128], lnvb[:, g:g + 1], bsbc[:, g, :], op0=ALU.mult, op1=ALU.add),
                    R=[self.PK(bk), "lnvb", "bsbc"], W=[("Bt", g)])
        self.modsP(self.ada_w[li], [(0, False), (1, True)], self.adabT, [self.modP[:, 0, :], self.modP[:, 1, :]])
        self.modsBC(li, 2, g1bc, adab)
        fw.barrier()
        HTh = self.A0[:, 0:8192].rearrange("p (k t) -> p k t", k=KC)
        UT = self.A0[:, 8192:16384].rearrange("p (k t) -> p k t", k=KC)
        w_in3 = self.w_in[li].rearrange("(k p) f -> p k f", p=128)
        w_o3 = self.w_o[li].rearrange("(k p) f -> p k f", p=128)
        for half in range(2):
            tiles = [half * 4 + i for i in range(4)]
            self.build_ht(tiles, self.modP[:, 1, :], self.modP[:, 0, :], HTh, 1)
            htk = [("HT", kc, 0) for kc in range(KC)]
            k = 0
            for n in range(4):
                def ld(slot, s, n=n):
                    fw.dma("pool", slot.rearrange("p (k f) -> p k f", k=KC), w_in3[:, :, D + n * 512: D + (n + 1) * 512], f"wr{s}", W=[("WR", s)])
                slot, wk = self.ring.next(("wv", li, half, n), ld)
                w = slot.rearrange("p (k f) -> p k f", k=KC)
                for i in range(4):
                    bk = k % 4
                    k += 1
                    pairs = [(HTh[:, kc, i * 128:(i + 1) * 128], w[:, kc, :]) for kc in range(KC)]
                    pairs.append((self.ones_bf[0:1, :], binv[0:1, n * 512:(n + 1) * 512]))
                    fw.mm(self.ps[bk][:, :], pairs, R=[wk, "ones", "binv"] + htk, W=[self.PK(bk)])
                    fw.op("act", lambda i=i, n=n, bk=bk: nc.scalar.activation(VB[:, i, n * 512:(n + 1) * 512], self.ps[bk][:, :], AF.Gelu_apprx_tanh),
                          R=[self.PK(bk)], W=[("VB", i, n)])
            for i in range(4):
                for n in range(4):
                    fw.op("dve", lambda i=i, n=n: nc.vector.bn_stats(st[:, n, :], VB[:, i, n * 512:(n + 1) * 512]), R=[("VB", i, n)], W=[("st", n)])
                fw.op("dve", lambda: nc.vector.bn_aggr(mv[:, 0:2], st[:, :, :]), R=[("st", c) for c in range(4)], W=["mv"])
                fw.op("dve", lambda: nc.vector.tensor_scalar(mv[:, 2:3], mv[:, 1:2], EPS, None, op0=ALU.add), R=["mv"], W=["mv2"])
                fw.op("act", lambda: nc.scalar.activation(mv[:, 2:3], mv[:, 2:3], AF.Sqrt), R=["mv2"], W=["mv2"])
                fw.op("dve", lambda: nc.vector.reciprocal(mv[:, 2:3], mv[:, 2:3]), R=["mv2"], W=["mv2"])
                fw.op("dve", lambda: nc.vector.scalar_tensor_tensor(mv[:, 3:4], mv[:, 0:1], -1.0, mv[:, 2:3], op0=ALU.mult, op1=ALU.mult),
                      R=["mv", "mv2"], W=["mv3"])
                fw.op("act", lambda i=i: nc.scalar.activation(VB[:, i, :], VB[:, i, :], AF.Identity, bias=mv[:, 3:4], scale=mv[:, 2:3]),
                      R=[("VB", i, n) for n in range(4)] + ["mv2", "mv3"], W=[("VB", i, n) for n in range(4)])
            for q in range(4):
                def ld(slot, s, q=q):
                    dst = slot.rearrange("p (c k f) -> p c k f", c=4, k=KC)
                    for c in range(4):
                        fw.dma("pool", dst[:, c], w_in3[:, :, (q * 4 + c) * 128:(q * 4 + c + 1) * 128], f"wr{s}", W=[("WR", s)])
                slot, wk = self.ring.next(("wu", li, half, q), ld)
                w = slot.rearrange("p (c k f) -> p c k f", c=4, k=KC)
                for c in range(4):
                    ch = q * 4 + c
                    bk = k % 4
                    k += 1
                    fw.mm(self.ps[bk][:, :], [(w[:, c, kc, :], HTh[:, kc, :]) for kc in range(KC)], R=[wk] + htk, W=[self.PK(bk)])
                    fw.op("act", lambda ch=ch, bk=bk: nc.scalar.activation(UT[:, ch, :], self.ps[bk][:, :], AF.Gelu_apprx_tanh, bias=binu[:, ch:ch + 1]),
                          R=[self.PK(bk), "binu"], W=[("UT", ch)])
            for i in range(4):
                for gq in range(4):
                    bk = k % 4
                    k += 1
                    for j in range(4):
                        g = gq * 4 + j
                        fw.mm1(self.ps[bk][:, j * 128:(j + 1) * 128], VB[:, i, g * 128:(g + 1) * 128], WST[:, g, :], start=True, stop=True,
                               R=[("VB", i, g // 4), ("WST", gq)], W=[self.PK(bk)], last=(j == 3))
                    tb = tmp[k % 2]
                    tk = ("tmp", k % 2)
                    for j in range(4):
                        g = gq * 4 + j
                        fw.op("dve", lambda g=g, j=j, bk=bk, tb=tb: nc.vector.scalar_tensor_tensor(
                            tb[:, j * 128:(j + 1) * 128], self.ps[bk][:, j * 128:(j + 1) * 128], lnvg[:, g:g + 1], Bt[:, g, :], op0=ALU.mult, op1=ALU.add),
                            R=[self.PK(bk), "lnvg", ("Bt", g)], W=[tk])
                    uview = UT[:, gq * 4:(gq + 1) * 4, i * 128:(i + 1) * 128]
                    fw.op("dve", lambda uview=uview, tb=tb: nc.vector.tensor_tensor(uview, tb.rearrange("p (g t) -> p g t", g=4), uview, op=ALU.mult),
                          R=[tk] + [("UT", gq * 4 + j) for j in range(4)], W=[("UT", gq * 4 + j) for j in range(4)])
            utk = [("UT", c) for c in range(KC)]
            for n in range(4):
                def ld(slot, s, n=n):
                    fw.dma("pool", slot.rearrange("p (k f) -> p k f", k=KC), w_o3[:, :, n * 512:(n + 1) * 512], f"wr{s}", W=[("WR", s)])
                slot, wk = self.ring.next(("wo", li, half, n), ld)
                w = slot.rearrange("p (k f) -> p k f", k=KC)
                for i in range(4):
                    bk = k % 4
                    k += 1
                    fw.mm(self.ps[bk][:, :], [(UT[:, kc, i * 128:(i + 1) * 128], w[:, kc, :]) for kc in range(KC)], R=[wk] + utk, W=[self.PK(bk)])
                    self.resid_evac(bk, tiles[i], n, g1bc, tmp[k % 2], ("tmp", k % 2))
        fw.barrier()
        ar.off = mark
        gbc = ar.f32(D)
        bbc = ar.f32(D)
        self.layer_norm(range(NT), self.ln_g[li, 0], self.ln_b[li, 0], gbc, bbc, st, mv)

    def moe(self, li):
        nc, fw, ar = self.nc, self.fw, self.arena
        fw.barrier()
        ar.off = 0
        g2bc = ar.f32(D)
        DT = [ar.f32(512), ar.f32(512)]
        G = ar.f32(NT * NE).rearrange("p (t e) -> p t e", t=NT)
        Gp = ar.f32(NT * NE).rearrange("p (t e) -> p t e", t=NT)
        bgu = ar.f32(NE * 8).rearrange("p (e c) -> p e c", e=NE)
        st = ar.f32(24).rearrange("p (c s) -> p c s", c=4)
        mv = ar.f32(4)
        mark = ar.off
        ACTT = [ar.bf16(4096).rearrange("p (j t) -> p j t", j=4) for _ in range(2)]
        TT = [ar.f32(512), ar.f32(512)]
        UU = [ar.f32(512), ar.f32(512)]
        ar.off = mark
        hf = [ar.f32(512), ar.f32(512)]
        wr32 = ar.f32(KC * 128).rearrange("p (k e) -> p k e", k=KC)
        bd32 = ar.f32(D)
        LTs = ar.f32(TOK)
        L = ar.f32(NT * NE).rearrange("p (t e) -> p t e", t=NT)
        MS = ar.f32(NT * NE).rearrange("p (t e) -> p t e", t=NT)
        m8 = ar.f32(8)
        sm = ar.f32(4)
        GT = ar.f32(TOK).rearrange("p (t k) -> p t k", t=NT)
        adab = [ar.f32(512), ar.f32(512)]
        brt = ar.f32(1)
        fw.dma("sp", self.adabT[:], self.ada_bT[li], "adabT", W=["adabT"])
        fw.op("pool", lambda: nc.gpsimd.memset(wr32, 0.0), W=["wr32"])
        fw.dma("sp", wr32[:, :, 0:NE], self.w_r[li].rearrange("(k p) e -> p k e", p=128), "wr32", R=["wr32"], W=["wr32"])
        fw.dma("sp", bd32[0:NE, :], self.b_dn[li], "bd32", W=["bd32"])
        fw.dma("sp", brt[0:NE, :], self.b_rT[li], "brt", W=["brt"])
        fw.dma("sp", bgu, self.b_guT[li], "bgu", W=["bgu"])
        import os
        PRO = int(os.environ.get("MOE_PRO", 99))
        if PRO <= 0:
            return
        fw.op("dve", lambda: nc.vector.tensor_scalar(bgu[:, :, 0:4], bgu[:, :, 0:4], SW_A, None, op0=ALU.mult), R=["bgu"], W=["bgu"])
        fw.op("dve", lambda: nc.vector.tensor_scalar(bgu[:, :, 4:8], bgu[:, :, 4:8], 1.0, None, op0=ALU.add), R=["bgu"], W=["bgu"])
        self.modsP(self.ada_w[li], [(3, False), (4, True)], self.adabT, [self.modP[:, 2, :], self.modP[:, 3, :]])
        if PRO <= 1:
            return
        self.modsBC(li, 5, g2bc, adab)
        HT = self.HT()
        if PRO <= 2:
            return
        self.build_ht(list(range(NT)), self.modP[:, 3, :], self.modP[:, 2, :], HT, 2, router=(hf, wr32, LTs))
        if PRO <= 3:
            return
        for tg in range(2):
            fw.op("act", lambda tg=tg: nc.scalar.activation(LTs[0:NE, tg * 512:(tg + 1) * 512], LTs[0:NE, tg * 512:(tg + 1) * 512], AF.Identity, bias=brt[0:NE, 0:1]),
                  R=[("LTs", tg), "brt"], W=[("LTs", tg)])
        for t in range(NT):
            fw.tr(self.ps[6][:, t * NE:(t + 1) * NE], LTs[0:NE, t * 128:(t + 1) * 128], self.ident[0:NE, 0:NE],
                  R=[("LTs", t // 4), "ident"], W=[self.PK(6)], last=(t == NT - 1))
        fw.op("dve", lambda: nc.vector.tensor_copy(L, self.ps[6][:, 0:NT * NE].rearrange("p (t e) -> p t e", t=NT)), R=[self.PK(6)], W=["L"])
        if PRO <= 4:
            return
        for t in range(NT):
            Lt, Mt, Gt, Gpt = L[:, t, :], MS[:, t, :], G[:, t, :], Gp[:, t, :]
            fw.op("dve", lambda Lt=Lt: nc.vector.max(m8, Lt), R=["L"], W=["m8"])
            fw.op("dve", lambda Lt=Lt, Mt=Mt: nc.vector.tensor_scalar(Mt, Lt, m8[:, 3:4], None, op0=ALU.is_ge), R=["L", "m8"], W=["MS"])
            fw.op("dve", lambda: nc.vector.tensor_scalar(sm[:, 0:1], m8[:, 0:1], -1.0, None, op0=ALU.mult), R=["m8"], W=["sm0"])
            fw.op("act", lambda Lt=Lt, Gt=Gt: nc.scalar.activation(Gt, Lt, AF.Exp, bias=sm[:, 0:1], scale=1.0), R=["L", "sm0"], W=["G"])
            fw.op("dve", lambda Gt=Gt, Mt=Mt: nc.vector.tensor_tensor(Gt, Gt, Mt, op=ALU.mult), R=["G", "MS"], W=["G"])
            fw.op("dve", lambda Gt=Gt: nc.vector.reduce_sum(sm[:, 1:2], Gt, axis=AX.X), R=["G"], W=["sm1"])
            fw.op("dve", lambda: nc.vector.reciprocal(sm[:, 2:3], sm[:, 1:2]), R=["sm1"], W=["sm2"])
            fw.op("dve", lambda Gt=Gt: nc.vector.tensor_scalar(Gt, Gt, sm[:, 2:3], None, op0=ALU.mult), R=["G", "sm2"], W=["G"])
            fw.op("dve", lambda Gt=Gt, Gpt=Gpt: nc.vector.tensor_scalar(Gpt, Gt, 1.0 / SW_A, None, op0=ALU.mult), R=["G"], W=["Gp"])
        if PRO <= 5:
            return
        for t in range(NT):
            bk = 6 + (t // 4)
            fw.tr(self.ps[bk][0:NE, (t % 4) * 128:(t % 4 + 1) * 128], G[:, t, :], self.ident[:], R=["G", "ident"], W=[self.PK(bk)], last=(t % 4 == 3))
        for h in range(2):
            fw.op("act", lambda h=h: nc.scalar.copy(GT[0:NE, h * 4:(h + 1) * 4, :], self.ps[6 + h][0:NE, :].rearrange("p (t k) -> p t k", t=4)),
                  R=[self.PK(6 + h)], W=[("GT", h)])
        if PRO <= 6:
            return
        k = 0
        for t in range(NT):
            for n in range(4):
                bk = 4 + k % 2
                fw.mm(self.ps[bk][:, :], [(GT[0:NE, t, :], bd32[0:NE, n * 512:(n + 1) * 512])], R=[("GT", t // 4), "bd32"], W=[self.PK(bk)])
                self.resid_evac(bk, t, n, g2bc, DT[k % 2], ("DT", k % 2))
                k += 1
        fw.barrier()
        w_gu = self.w_gu[li]
        w_dn = self.w_dn[li]
        htk = [[("HT", kc, th) for kc in range(KC)] for th in range(2)]

        def gu_unit(e, j, state):
            if j % 2 == 0:
                def ld(slot, s, e=e, j=j):
                    dst = slot.rearrange("p (a h k f) -> p a h k f", a=2, h=2, k=KC)
                    src = w_gu[e].rearrange("(k p) f -> p k f", p=128)
                    for a in range(2):
                        for h in range(2):
                            c0 = h * 512 + (j + a) * 128
                            fw.dma("pool", dst[:, a, h], src[:, :, c0:c0 + 128], f"wr{s}", W=[("WR", s)])
                slot, wk = self.ring.next(("gu", li, e, j), ld)
                state["gu"] = (slot.rearrange("p (a h k f) -> p a h k f", a=2, h=2, k=KC), wk)
            w, wk = state["gu"]
            a = j % 2
            eb = e % 2
            for th in range(2):
                fw.mm(self.ps[th * 2][:, :], [(w[:, a, 0, kc, :], HT[:, kc, th * 512:(th + 1) * 512]) for kc in range(KC)],
                      R=[wk] + htk[th], W=[self.PK(th * 2)])
                fw.mm(self.ps[th * 2 + 1][:, :], [(w[:, a, 1, kc, :], HT[:, kc, th * 512:(th + 1) * 512]) for kc in range(KC)],
                      R=[wk] + htk[th], W=[self.PK(th * 2 + 1)])
                fw.op("act", lambda th=th: nc.scalar.activation(TT[th], self.ps[th * 2][:, :], AF.Silu, bias=bgu[:, e, j:j + 1], scale=SW_A),
                      R=[self.PK(th * 2), "bgu"], W=[("TT", th)])
                fw.op("act", lambda th=th: nc.scalar.activation(UU[th], self.ps[th * 2 + 1][:, :], AF.Identity, bias=bgu[:, e, 4 + j:5 + j]),
                      R=[self.PK(th * 2 + 1), "bgu"], W=[("UU", th)])
                fw.op("dve", lambda th=th: nc.vector.tensor_scalar(UU[th], UU[th], 8.0, -6.0, op0=ALU.min, op1=ALU.max), R=[("UU", th)], W=[("UU", th)])
                fw.op("dve", lambda th=th: nc.vector.scalar_tensor_tensor(ACTT[eb][:, j, th * 512:(th + 1) * 512], TT[th], C7, UU[th], op0=ALU.min, op1=ALU.mult),
                      R=[("TT", th), ("UU", th)], W=[("ACTT", eb, j)])

        def down_unit(e, state):
            def ld(slot, s, e=e):
                fw.dma("pool", slot.rearrange("p (j n) -> p j n", j=4), w_dn[e].rearrange("(j p) n -> p j n", p=128), f"wr{s}", W=[("WR", s)])
            slot, wk = self.ring.next(("dn", li, e), ld)
            w = slot.rearrange("p (j n) -> p j n", j=4)
            eb = e % 2
            k = state.get("dk", 0)
            for t in range(NT):
                for n in range(4):
                    bk = 4 + k % 2
                    dt_ = DT[k % 2]
                    dk = ("DT", k % 2)
                    k += 1
                    fw.mm(self.ps[bk][:, :], [(ACTT[eb][:, j, t * 128:(t + 1) * 128], w[:, j, n * 512:(n + 1) * 512]) for j in range(4)],
                          R=[wk] + [("ACTT", eb, j) for j in range(4)], W=[self.PK(bk)])
                    fw.op("dve", lambda bk=bk, dt_=dt_, t=t, n=n: nc.vector.scalar_tensor_tensor(
                        dt_, self.ps[bk][:, :], Gp[:, t, e:e + 1], g2bc[:, n * 512:(n + 1) * 512], op0=ALU.mult, op1=ALU.mult),
                        R=[self.PK(bk), "Gp", ("gbc", n)], W=[dk])
                    xs = self.X[:, t, n * 512:(n + 1) * 512]
                    fw.op("dve", lambda xs=xs, dt_=dt_: nc.vector.tensor_tensor(xs, xs, dt_, op=ALU.add), R=[("X", t), dk], W=[("X", t)])
            state["dk"] = k

        state = {}
        import os
        nexp = int(os.environ.get("MOE_NEXP", NE))
        for e in range(nexp):
            gu_unit(e, 0, state)
            if e > 0:
                down_unit(e - 1, state)
            for j in range(1, 4):
                gu_unit(e, j, state)
        if nexp > 0:
            down_unit(nexp - 1, state)
        fw.barrier()
        ar.off = mark
        gbc = ar.f32(D)
        bbc = ar.f32(D)
        self.layer_norm(range(NT), self.ln_g[li, 1], self.ln_b[li, 1], gbc, bbc, st, mv)

    def kv(self):
        nc, fw, ar = self.nc, self.fw, self.arena
        fw.barrier()
        ar.off = 0
        KTs = [ar.bf16(TOK), ar.bf16(TOK)]
        Vs = [ar.bf16(512), ar.bf16(512)]
        kms = ar.f32(64).rearrange("p (h b) -> p h b", h=16)
        kvb = ar.f32(32)
        modKV = ar.f32(32).rearrange("p (v k) -> p v k", v=2)
        fw.dma("sp", kvb, self.kv_ada_bT, "kvb", W=["adabT"])
        self.modsP(self.kv_ada_w, [(0, False), (1, True)], kvb, [modKV[:, 0, :], modKV[:, 1, :]])
        HT = self.HT()
        self.build_ht(list(range(NT)), modKV[:, 1, :], modKV[:, 0, :], HT, 2)
        htk = [[("HT", kc, th) for kc in range(KC)] for th in range(2)]
        w3 = self.w_kv.rearrange("(k p) f -> p k f", p=128)
        k = 0
        for q in range(4):
            def ld(slot, s, q=q):
                dst = slot.rearrange("p (c k f) -> p c k f", c=4, k=KC)
                for c in range(4):
                    fw.dma("pool", dst[:, c], w3[:, :, (q * 4 + c) * 128:(q * 4 + c + 1) * 128], f"wr{s}", W=[("WR", s)])
            slot, wk = self.ring.next(("wk", q), ld)
            w = slot.rearrange("p (c k f) -> p c k f", c=4, k=KC)
            for c in range(4):
                h = q * 4 + c
                kb = KTs[h % 2]
                kk = ("KTs", h % 2)
                for th in range(2):
                    bk = k % 4
                    k += 1
                    fw.mm(self.ps[bk][:, :], [(w[:, c, kc, :], HT[:, kc, th * 512:(th + 1) * 512]) for kc in range(KC)], R=[wk] + htk[th], W=[self.PK(bk)])
                    fw.op("act", lambda kb=kb, th=th, bk=bk: nc.scalar.copy(kb[:, th * 512:(th + 1) * 512], self.ps[bk][:, :]), R=[self.PK(bk)], W=[kk])
                    fw.op("dve", lambda h=h, th=th, bk=bk: nc.vector.tensor_reduce(kms[:, h, th * 2:(th + 1) * 2], self.ps[bk][:, :].rearrange("p (b s) -> p b s", b=2),
                                                                                   op=ALU.add, axis=AX.X), R=[self.PK(bk)], W=["kms"])
                self.out_toks.append(fw.dma("sp", self.kt_o[h], kb, f"kto{h % 2}", R=[kk]))
        fw.op("dve", lambda: nc.vector.tensor_scalar(kms, kms, 1.0 / 256.0, None, op0=ALU.mult), R=["kms"], W=["kms"])
        self.out_toks.append(fw.dma("sp", self.km_o, kms, "kmo", R=["kms"]))
        k2 = 0
        for n in range(4):
            def ld(slot, s, n=n):
                fw.dma("pool", slot.rearrange("p (k f) -> p k f", k=KC), w3[:, :, D + n * 512: D + (n + 1) * 512], f"wr{s}", W=[("WR", s)])
            slot, wk = self.ring.next(("wvv", n), ld)
            w = slot.rearrange("p (k f) -> p k f", k=KC)
            for t in range(NT):
                bk = k % 4
                k += 1
                vb = Vs[k2 % 2]
                vk = ("Vs", k2 % 2)
                k2 += 1
                fw.mm(self.ps[bk][:, :], [(HT[:, kc, t * 128:(t + 1) * 128], w[:, kc, :]) for kc in range(KC)], R=[wk] + htk[t // 4], W=[self.PK(bk)])
                fw.op("act", lambda vb=vb, bk=bk: nc.scalar.copy(vb, self.ps[bk][:, :]), R=[self.PK(bk)], W=[vk])
                self.out_toks.append(fw.dma("sp", self.v_o[t * 128:(t + 1) * 128, n * 512:(n + 1) * 512], vb, f"vo{k2 % 2}", R=[vk]))

    def attn(self, li):
        nc, fw, ar = self.nc, self.fw, self.arena
        fw.barrier()
        ar.off = 0
        st = ar.f32(24).rearrange("p (c s) -> p c s", c=4)
        mv = ar.f32(4)
        mark = ar.off
        QT = ar.bf16(KC * TOK).rearrange("p (h t) -> p h t", h=16)
        KMb = ar.bf16(256).rearrange("p (h n) -> p h n", h=16)
        PM = ar.f32(128)
        KTo = [ar.bf16(TOK) for _ in range(2)]
        Vo = [ar.bf16(TOK).rearrange("p (t d) -> p t d", t=NT) for _ in range(2)]
        MBT = [ar.bf16(TOK) for _ in range(2)]
        GS = ar.f32(128)
        SEL = ar.f32(128)
        m8 = ar.f32(64).rearrange("p (t e) -> p t e", t=NT)
        thr = ar.f32(8)
        PT = [ar.bf16(256) for _ in range(3)]
        RI = [ar.f32(256) for _ in range(2)]
        EN = ar.bf16(16 * 128).rearrange("p (n k) -> p n k", n=16)
        TRI2 = ar.bf16(256)
        TRI3 = ar.bf16(256)
        identb = ar.bf16(128)
        fw.dma("sp", self.adabT[:], self.ada_bT[li], "adabT", W=["adabT"])
        fw.dma("pool", KMb, self.km_all, "kmb", W=["KMb"])
        fw.op("pool", lambda: nc.gpsimd.memset(EN[0:16], 1.0), W=["EN"])
        fw.op("pool", lambda: nc.gpsimd.affine_select(EN[0:16], EN[0:16], pattern=[[-1, 16], [0, 128]], compare_op=ALU.is_equal,
                                                      fill=0.0, base=0, channel_multiplier=1), R=["EN"], W=["EN"])
        fw.op("pool", lambda: nc.gpsimd.memset(TRI2, 0.0), W=["TRI2"])
        fw.op("pool", lambda: nc.gpsimd.affine_select(TRI2[:, 0:128], TRI2[:, 0:128], pattern=[[1, 128]], compare_op=ALU.is_ge,
                                                      fill=NEG, base=0, channel_multiplier=-1), R=["TRI2"], W=["TRI2"])
        fw.op("pool", lambda: nc.gpsimd.memset(TRI3, NEG), W=["TRI3"])
        fw.op("pool", lambda: nc.gpsimd.tensor_copy(TRI3[:, 128:256], TRI2[:, 0:128]), R=["TRI2", "TRI3"], W=["TRI3"])
        fw.op("dve", lambda: nc.vector.tensor_copy(identb, self.ident[:]), R=["ident"], W=["identb"])
        self.modsP(self.ada_w[li], [(0, False), (1, True)], self.adabT, [self.modP[:, 0, :], self.modP[:, 1, :]])
        HT = self.HT()
        self.build_ht(list(range(NT)), self.modP[:, 1, :], self.modP[:, 0, :], HT, 2)
        htk = [[("HT", kc, th) for kc in range(KC)] for th in range(2)]
        wq3 = self.w_q[li].rearrange("(k p) f -> p k f", p=128)
        k = 0
        for q in range(4):
            def ld(slot, s, q=q):
                dst = slot.rearrange("p (c k f) -> p c k f", c=4, k=KC)
                for c in range(4):
                    fw.dma("pool", dst[:, c], wq3[:, :, (q * 4 + c) * 128:(q * 4 + c + 1) * 128], f"wr{s}", W=[("WR", s)])
            slot, wk = self.ring.next(("wq", li, q), ld)
            w = slot.rearrange("p (c k f) -> p c k f", c=4, k=KC)
            for c in range(4):
                h = q * 4 + c
                for th in range(2):
                    bk = k % 4
                    k += 1
                    fw.mm(self.ps[bk][:, :], [(w[:, c, kc, :], HT[:, kc, th * 512:(th + 1) * 512]) for kc in range(KC)], R=[wk] + htk[th], W=[self.PK(bk)])
                    fw.op("act", lambda h=h, th=th, bk=bk: nc.scalar.activation(QT[:, h, th * 512:(th + 1) * 512], self.ps[bk][:, :], AF.Identity, scale=128.0 ** -0.5),
                          R=[self.PK(bk)], W=[("QT", h)])
        fw.barrier()
        OT = self.HT()
        fw.dma("sp", PM, self.pastm.rearrange("p t n -> p (t n)"), "pm", W=["PM"])
        sk = 0
        for h in range(16):
            hb = h % 2
            def ld(slot, s, h=h, kta=self.kt_all, va=self.v_all):
                fw.dma("pool", slot[:, 0:4096], kta[h], f"wr{s}", W=[("WR", s)])
                fw.dma("pool", slot[:, 4096:8192].rearrange("p (t d) -> p t d", t=32), va[h], f"wr{s}", W=[("WR", s)])
            slot, wk = self.ring.next(("kvh", li, h), ld)
            KTa = slot[:, 0:4096]
            Va = slot[:, 4096:8192].rearrange("p (t d) -> p t d", t=32)
            fw.dma("sp", KTo[hb], self.kt_own[h], f"kto{hb}", W=[("KTo", hb)])
            fw.dma("sp", Vo[hb], self.v_own[h], f"vo{hb}", W=[("Vo", hb)])
            for t in range(NT):
                fw.mm1(self.ps[6][:, t * 16:(t + 1) * 16], QT[:, h, t * 128:(t + 1) * 128], KMb[:, h, :], start=True, stop=True,
                       R=[("QT", h), "KMb"], W=[self.PK(6)], last=(t == NT - 1))
            fw.op("dve", lambda: nc.vector.tensor_tensor(GS, self.ps[6][:, 0:128], PM, op=ALU.add), R=[self.PK(6), "PM"], W=["GS"])
            for t in range(NT):
                fw.op("dve", lambda t=t: nc.vector.max(m8[:, t, :], GS[:, t * 16:(t + 1) * 16]), R=["GS"], W=["m8"])
            fw.op("dve", lambda: nc.vector.tensor_scalar(thr, m8[:, :, 2], -1e29, None, op0=ALU.max), R=["m8"], W=["thr"])
            for t in range(NT):
                fw.op("dve", lambda t=t: nc.vector.tensor_scalar(SEL[:, t * 16:(t + 1) * 16], GS[:, t * 16:(t + 1) * 16], thr[:, t:t + 1], None, op0=ALU.is_ge),
                      R=["GS", "thr"], W=["SEL"])
            fw.op("dve", lambda: nc.vector.tensor_scalar(SEL, SEL, -1.0, -NEG, op0=ALU.add, op1=ALU.mult), R=["SEL"], W=["SEL"])
            for t in range(NT):
                bk = 7
                fw.tr(self.ps[bk][0:16, (t % 4) * 128:(t % 4 + 1) * 128], SEL[:, t * 16:(t + 1) * 16], self.ident[:], R=["SEL", "ident"], W=[self.PK(bk)], last=(t % 4 == 3))
                if t % 4 == 3:
                    tg = t // 4
                    fw.op("act", lambda tg=tg: nc.scalar.copy(MBT[hb][0:16, tg * 512:(tg + 1) * 512], self.ps[7][0:16, :]), R=[self.PK(7)], W=[("MBT", hb, tg)])
            for lb in range(4):
                q0 = lb * 256
                qv = QT[:, h, q0:q0 + 256]
                items = []
                for n in range(4 * lb + 3):
                    for kh in range(2):
                        kt = n * 2 + kh
                        items.append((KTa[:, kt * 128:(kt + 1) * 128], EN[0:16, n, :], MBT[hb][0:16, q0:q0 + 256], Va[:, kt, :], 256, 0,
                                      [wk, ("MBT", hb, lb // 2), "EN"]))
                items.append((KTo[hb][:, q0:q0 + 128], identb, TRI2, Vo[hb][:, 2 * lb, :], 256, 0, [("KTo", hb), ("Vo", hb), "identb", "TRI2"]))
                items.append((KTo[hb][:, q0 + 128:q0 + 256], identb, TRI3, Vo[hb][:, 2 * lb + 1, :], 256, 0, [("KTo", hb), ("Vo", hb), "identb", "TRI3"]))
                ni = len(items)
                for ii, (kl, ml, mr, vt, ncol, c0, keys) in enumerate(items):
                    sb = sk % 2
                    pb = sk % 3
                    sk += 1
                    S = self.ps[sb][:, 0:ncol]
                    fw.mm1(S, kl, qv[:, c0:c0 + ncol], start=True, stop=False, R=keys + [("QT", h)], W=[self.PK(sb)])
                    fw.mm1(S, ml, mr, start=False, stop=True, R=keys, W=[self.PK(sb)], last=True)
                    fw.op("act", lambda S=S, pb=pb, ncol=ncol: nc.scalar.activation(PT[pb][:, 0:ncol], S, AF.Exp), R=[self.PK(sb)], W=[("PT", pb)])
                    fw.mm1(self.ps[2][:, c0:c0 + ncol], vt, PT[pb][:, 0:ncol], start=(ii == 0), stop=(ii == ni - 1), R=keys + [("PT", pb)], W=[self.PK(2)])
                    fw.mm1(self.ps[3][:, c0:c0 + ncol], self.ones_bf[:], PT[pb][:, 0:ncol], start=(ii == 0), stop=(ii == ni - 1), R=["ones", ("PT", pb)], W=[self.PK(3)],
                           last=True)
                rb = lb % 2
                fw.op("dve", lambda rb=rb: nc.vector.reciprocal(RI[rb], self.ps[3][:, 0:256]), R=[self.PK(3)], W=[("RI", rb)])
                fw.op("dve", lambda rb=rb, q0=q0, h=h: nc.vector.tensor_tensor(OT[:, h, q0:q0 + 256], self.ps[2][:, 0:256], RI[rb], op=ALU.mult),
                      R=[self.PK(2), ("RI", rb)], W=[("OT", h)])
        fw.barrier()
        ar.off = mark
        g1bc = ar.f32(D)
        adab = [ar.f32(512), ar.f32(512)]
        tmp = [ar.f32(512), ar.f32(512)]
        self.modsBC(li, 2, g1bc, adab)
        wo3 = self.w_ao[li].rearrange("(k p) f -> p k f", p=128)
        otk = [("OT", h) for h in range(16)]
        k = 0
        for n in range(4):
            def ld(slot, s, n=n):
                fw.dma("pool", slot.rearrange("p (k f) -> p k f", k=KC), wo3[:, :, n * 512:(n + 1) * 512], f"wr{s}", W=[("WR", s)])
            slot, wk = self.ring.next(("wao", li, n), ld)
            w = slot.rearrange("p (k f) -> p k f", k=KC)
            for t in range(NT):
                bk = k % 4
                k += 1
                fw.mm(self.ps[bk][:, :], [(OT[:, hh, t * 128:(t + 1) * 128], w[:, hh, :]) for hh in range(16)], R=[wk] + otk, W=[self.PK(bk)])
                self.resid_evac(bk, t, n, g1bc, tmp[k % 2], ("tmp", k % 2))
        fw.barrier()
        gbc = ar.f32(D)
        bbc = ar.f32(D)
        self.layer_norm(range(NT), self.ln_g[li, 0], self.ln_b[li, 0], gbc, bbc, st, mv)

    def phases(self):
        ph = []
        if self.stage == "A":
            for li in range(2):
                ph.append(lambda li=li: self.gmlp(li))
                ph.append(lambda li=li: self.moe(li))
            ph.append(self.kv)
        else:
            for li in range(2):
                ph.append(lambda li=li: self.attn(li))
                ph.append(lambda li=li: self.moe(li))
        return ph

    def emit(self):
        ph = self.phases()
        if self.stop_after is not None:
            ph = ph[:self.stop_after]
        fw = self.fw
        fw.dry = True
        for p in range(self.npass):
            self.set_pass(p)
            for f in ph:
                f()
        fw.dry = False
        self.ring.reset()
        for p in range(self.npass):
            self.set_pass(p)
            if p > 0:
                fw.barrier()
            self.init_consts(first=(p == 0))
            for f in ph:
                f()
            fw.barrier()
            for t in range(NT):
                self.out_toks.append(fw.dma("sp", self.y_d[t * 128:(t + 1) * 128, :], self.X[:, t, :], f"x{t}", R=[("X", t)]))
        fw.finish(self.out_toks)
        return self.nc


BLOCKS = lambda j: [j, 7 - j, 8 + j, 15 - j]


def _common_maps(inp, layers):
    l0 = layers[0]
    sl = slice(l0, l0 + 2)
    f = lambda a: np.ascontiguousarray(a, dtype=np.float32)
    m = {
        "ada_w": f(inp["ada_w"][sl]), "ada_b": f(inp["ada_b"][sl]),
        "ada_bT": f(inp["ada_b"][sl].reshape(2, 96, 128).transpose(0, 2, 1)),
        "ln_g": f(inp["ln_g"][sl]), "ln_b": f(inp["ln_b"][sl]),
        "w_r": f(inp["moe_w_router"][sl]), "b_rT": f(inp["moe_b_router"][sl].reshape(2, NE, 1)),
        "w_gu": f(inp["moe_w_gu"][sl]),
        "b_guT": f(inp["moe_b_gu"][sl].reshape(2, NE, 8, 128).transpose(0, 3, 1, 2)),
        "w_dn": f(inp["moe_w_down"][sl]), "b_dn": f(inp["moe_b_down"][sl]),
    }
    return m


def _cl(c, b):
    return np.ascontiguousarray(c[b].reshape(KC, 128).T, dtype=np.float32)


_PROG_CACHE = {}


def _get_prog(stage, stop_after=None, npass=1):
    key = (stage, stop_after, npass)
    if key not in _PROG_CACHE:
        p = Prog(stage, stop_after, npass)
        p.emit()
        _PROG_CACHE[key] = p
    return _PROG_CACHE[key]


def _vx(inp, v):
    b, j = v // 4, v % 4
    return np.ascontiguousarray(np.concatenate([inp["x"][b, g * 256:(g + 1) * 256] for g in BLOCKS(j)], axis=0), dtype=np.float32)


def run_stage_a(inp, stop_after=None, groups=None):
    f = lambda a: np.ascontiguousarray(a, dtype=np.float32)
    groups = groups or [[c, c + 4] for c in range(4)]
    npass = len(groups[0])
    com = _common_maps(inp, [0, 1])
    com.update({
        "w_in": f(inp["gm_w_in"]), "b_in": f(inp["gm_b_in"]),
        "b_inuT": f(inp["gm_b_in"][:, :D].reshape(2, KC, 128).transpose(0, 2, 1)),
        "lnv_gT": f(inp["gm_lnv_g"].reshape(2, KC, 128).transpose(0, 2, 1)),
        "lnv_bT": f(inp["gm_lnv_b"].reshape(2, KC, 128).transpose(0, 2, 1)),
        "w_s": f(inp["gm_w_s"]), "b_s": f(inp["gm_b_s"].reshape(2, 16 * 128)),
        "w_o": f(inp["gm_w_out"]), "kv_ada_w": f(inp["kv_ada_w"]),
        "kv_ada_bT": f(inp["kv_ada_b"].reshape(32, 128).T), "w_kv": f(inp["w_kv"]),
    })
    maps = []
    for grp in groups:
        m = dict(com)
        for p, v in enumerate(grp):
            m[f"x{p}"] = _vx(inp, v)
            m[f"cl{p}"] = _cl(inp["c"], v // 4)
        maps.append(m)
    prog = _get_prog("A", stop_after, npass)
    res = run_bass_kernel_spmd(prog.nc, maps, core_ids=list(range(len(maps)))).results
    ra = {}
    for c, grp in enumerate(groups):
        for p, v in enumerate(grp):
            ra[v] = {k: np.asarray(res[c][f"{k}{p}"]) for k in ("y", "kt_o", "v_o", "km_o")}
    return ra


def run_stage_b(inp, ra, stop_after=None, groups=None):
    f = lambda a: np.ascontiguousarray(a, dtype=np.float32)
    groups = groups or [[c, c + 4] for c in range(4)]
    npass = len(groups[0])
    com = _common_maps(inp, [2, 3])
    com.update({"w_q": f(inp["attn_w_q"]), "w_ao": f(inp["attn_w_out"])})
    bf = ml_dtypes.bfloat16
    per_batch = {}
    for b in sorted(set(v // 4 for grp in groups for v in grp)):
        kt_all = np.zeros((16, 128, 4096), dtype=bf)
        v_tok = np.zeros((4096, D), dtype=bf)
        km_all = np.zeros((128, 16, 16), dtype=np.float32)
        for j in range(4):
            r = ra[b * 4 + j]
            for lb, g in enumerate(BLOCKS(j)):
                kt_all[:, :, g * 256:(g + 1) * 256] = r["kt_o"][:, :, lb * 256:(lb + 1) * 256]
                v_tok[g * 256:(g + 1) * 256] = r["v_o"][lb * 256:(lb + 1) * 256]
                km_all[:, :, g] = r["km_o"][:, :, lb]
        v_all = np.ascontiguousarray(v_tok.reshape(32, 128, 16, 128).transpose(2, 1, 0, 3))
        per_batch[b] = (kt_all, v_all, km_all)
    maps = []
    for grp in groups:
        m = dict(com)
        for p, v in enumerate(grp):
            b, j = v // 4, v % 4
            r = ra[v]
            m[f"x{p}"] = f(r["y"])
            m[f"cl{p}"] = _cl(inp["c"], b)
            m[f"kt_all{p}"], m[f"v_all{p}"], m[f"km_all{p}"] = per_batch[b]
            m[f"kt_own{p}"] = np.ascontiguousarray(r["kt_o"])
            m[f"v_own{p}"] = np.ascontiguousarray(np.asarray(r["v_o"]).reshape(NT, 128, 16, 128).transpose(2, 1, 0, 3))
            pm = np.zeros((128, NT, 16), dtype=np.float32)
            for lb, g in enumerate(BLOCKS(j)):
                pm[:, 2 * lb:2 * lb + 2, g:] = -1e30
            m[f"pastm{p}"] = pm
        maps.append(m)
    prog = _get_prog("B", stop_after, npass)
    res = run_bass_kernel_spmd(prog.nc, maps, core_ids=list(range(len(maps)))).results
    rb = {}
    for c, grp in enumerate(groups):
        for p, v in enumerate(grp):
            rb[v] = {"y": np.asarray(res[c][f"y{p}"])}
    return rb


def kernel(**inp):
    inp = {k: np.asarray(v) for k, v in inp.items()}
    ra = run_stage_a(inp)
    rb = run_stage_b(inp, ra)
    out = np.zeros((2, 4096, D), dtype=np.float32)
    for v in range(8):
        b, j = v // 4, v % 4
        for lb, g in enumerate(BLOCKS(j)):
            out[b, g * 256:(g + 1) * 256] = rb[v]["y"][lb * 256:(lb + 1) * 256]
    return out
```

```python
import math
import numpy as np
import ml_dtypes
import concourse.bass as bass
import concourse.mybir as mybir
from concourse.bass_utils import run_bass_kernel_spmd

F32 = mybir.dt.float32
BF16 = mybir.dt.bfloat16
AF = mybir.ActivationFunctionType
ALU = mybir.AluOpType
AX = mybir.AxisListType

D = 2048
KC = 16
NT = 8
TOK = 1024
NE = 32
ALPHA = 8.0 ** 0.25
EPS = 1e-5
SW_A = 1.702
C7 = SW_A * 7.0 / (1.0 + math.exp(-SW_A * 7.0))
NEG = -30000.0
SEM_CAP = 30000
NSLOT = 3


class Eng:
    def __init__(self, fw, name, handle):
        self.fw, self.name, self.h = fw, name, handle
        self.nsem, self.cnt, self.waited = 0, 0, {}
        self.pend_r, self.pend_w = [], []
        self._newsem()

    def _newsem(self):
        self.sem = self.fw.nc.alloc_semaphore(f"{self.name}_e{self.nsem}")
        self.nsem += 1
        self.cnt = 0

    def wait(self, tok):
        sem, val = tok
        if self.waited.get(sem.name, 0) >= val:
            return
        self.h.wait_ge(sem, val)
        self.waited[sem.name] = val

    def mark(self, ins):
        if self.cnt >= SEM_CAP:
            self._newsem()
        ins.then_inc(self.sem, 1)
        self.cnt += 1
        return (self.sem, self.cnt)


class FW:
    def __init__(self, nc):
        self.nc = nc
        self.dry = False
        self.E = {"pe": Eng(self, "pe", nc.tensor), "act": Eng(self, "act", nc.scalar),
                  "dve": Eng(self, "dve", nc.vector), "pool": Eng(self, "pool", nc.gpsimd),
                  "sp": Eng(self, "sp", nc.sync)}
        self.lw, self.rd, self.dsem = {}, {}, {}
        self.ninst = 0

    @staticmethod
    def _px(R, W):
        Rp = [k for k in R if isinstance(k, tuple) and k[0] == "ps"]
        if Rp:
            R = [k for k in R if not (isinstance(k, tuple) and k[0] == "ps")]
            W = list(W) + Rp
        return list(R), list(W)

    def _deps(self, eng, R, W):
        e = self.E[eng]
        pe = self.E["pe"]
        if eng != "pe" and pe.pend_r:
            for k in W:
                if k in pe.pend_r:
                    raise RuntimeError(f"write to {k} on {eng} while PE read has no token yet")
        for k in R:
            t = self.lw.get(k)
            if t is not None and not (t[1] == "pe" and eng == "pe"):
                e.wait(t[0])
        for k in W:
            t = self.lw.get(k)
            if t is not None and not (t[1] == "pe" and eng == "pe") and t[1] != getattr(self, "_skip_src", None):
                e.wait(t[0])
            for t in self.rd.get(k, ()):
                if not (t[1] == "pe" and eng == "pe"):
                    e.wait(t[0])

    def _commit(self, src, tok, R, W):
        for k in W:
            self.lw[k] = (tok, src)
            self.rd[k] = []
        for k in R:
            lst = self.rd.setdefault(k, [])
            lst[:] = [x for x in lst if x[1] != src]
            lst.append((tok, src))

    def _pe_done(self, ins, R, W):
        e = self.E["pe"]
        tok = e.mark(ins)
        R = list(R) + e.pend_r
        W = list(W) + e.pend_w
        e.pend_r, e.pend_w = [], []
        self._commit("pe", tok, R, W)
        return tok

    def op(self, eng, fn, R=(), W=()):
        if self.dry:
            return None
        R, W = self._px(R, W)
        self._deps(eng, R, W)
        ins = fn()
        self.ninst += 1
        tok = self.E[eng].mark(ins)
        self._commit(eng, tok, R, W)
        return tok

    def mm(self, out, pairs, R=(), W=()):
        if self.dry:
            return None
        self._deps("pe", R, W)
        n = len(pairs)
        ins = None
        for i, (a, b) in enumerate(pairs):
            ins = self.nc.tensor.matmul(out, a, b, start=(i == 0), stop=(i == n - 1))
        self.ninst += n
        return self._pe_done(ins, R, W)

    def mm1(self, out, a, b, start, stop, R=(), W=(), last=False):
        if self.dry:
            return None
        self._deps("pe", R, W)
        ins = self.nc.tensor.matmul(out, a, b, start=start, stop=stop)
        self.ninst += 1
        if last:
            return self._pe_done(ins, R, W)
        e = self.E["pe"]
        e.pend_r += list(R)
        e.pend_w += list(W)
        return None

    def tr(self, out, in_, ident, R=(), W=(), last=True):
        if self.dry:
            return None
        self._deps("pe", R, W)
        ins = self.nc.tensor.transpose(out, in_, ident)
        self.ninst += 1
        if last:
            return self._pe_done(ins, R, W)
        e = self.E["pe"]
        e.pend_r += list(R)
        e.pend_w += list(W)
        return None

    def dma(self, q, out, in_, chan, R=(), W=()):
        if self.dry:
            return None
        self._skip_src = "dma:" + chan
        self._deps(q, R, W)
        self._skip_src = None
        if chan not in self.dsem:
            self.dsem[chan] = [self.nc.alloc_semaphore(f"d_{chan}"), 0]
        ds = self.dsem[chan]
        self.E[q].h.dma_start(out=out, in_=in_).then_inc(ds[0], 16)
        self.ninst += 1
        ds[1] += 16
        tok = (ds[0], ds[1])
        self._commit("dma:" + chan, tok, R, W)
        return tok

    def barrier(self):
        if self.dry:
            return
        best = {}
        def add(tok):
            sem, val = tok
            if best.get(sem.name, (None, 0))[1] < val:
                best[sem.name] = (sem, val)
        for (t, s) in self.lw.values():
            add(t)
        for lst in self.rd.values():
            for (t, s) in lst:
                add(t)
        for en in ("pe", "act", "dve", "pool", "sp"):
            for tok in best.values():
                self.E[en].wait(tok)

    def finish(self, toks):
        for t in toks:
            if t is not None:
                self.E["sp"].wait(t)


class Ring:
    def __init__(self, fw, wr):
        self.fw, self.wr = fw, wr
        self.plan, self.cons, self.issued = [], 0, 0

    def reset(self):
        self.cons, self.issued = 0, 0

    def next(self, tag, loader):
        if self.fw.dry:
            self.plan.append((tag, loader))
            i = len(self.plan) - 1
            return self.wr[:, i % NSLOT, :], ("WR", i % NSLOT)
        i = self.cons
        assert self.plan[i][0] == tag, (self.plan[i][0], tag)
        while self.issued < min(len(self.plan), i + NSLOT):
            k = self.issued
            s = k % NSLOT
            self.plan[k][1](self.wr[:, s, :], s)
            self.issued += 1
        self.cons += 1
        return self.wr[:, i % NSLOT, :], ("WR", i % NSLOT)


class Arena:
    def __init__(self, t, n):
        self.t, self.n, self.off = t, n, 0

    def f32(self, n):
        assert self.off + n <= self.n, ("arena overflow", self.off, n, self.n)
        v = self.t[:, self.off:self.off + n]
        self.off += n
        return v

    def bf16(self, n):
        m = (n + 1) // 2
        return self.f32(m).bitcast(BF16)[:, 0:n]


class Prog:
    def __init__(self, stage, stop_after=None, npass=1):
        self.npass = npass
        self.stage = stage
        self.layers = [0, 1] if stage == "A" else [2, 3]
        self.stop_after = stop_after
        nc = self.nc = bass.Bass("TRN2", target_bir_lowering=False)
        self.fw = FW(nc)
        dt = lambda name, shape, ty=F32, kind="ExternalInput": nc.dram_tensor(name, list(shape), ty, kind=kind).ap()
        self.io = [dict() for _ in range(npass)]
        for p in range(npass):
            self.io[p]["x_d"] = dt(f"x{p}", [TOK, D])
            self.io[p]["cl_d"] = dt(f"cl{p}", [128, KC])
        self.ada_w = dt("ada_w", [2, D, 6 * D])
        self.ada_b = dt("ada_b", [2, 6 * D])
        self.ada_bT = dt("ada_bT", [2, 128, 96])
        self.ln_g = dt("ln_g", [2, 2, D])
        self.ln_b = dt("ln_b", [2, 2, D])
        self.w_r = dt("w_r", [2, D, NE])
        self.b_rT = dt("b_rT", [2, NE, 1])
        self.w_gu = dt("w_gu", [2, NE, D, 1024])
        self.b_guT = dt("b_guT", [2, 128, NE, 8])
        self.w_dn = dt("w_dn", [2, NE, 512, D])
        self.b_dn = dt("b_dn", [2, NE, D])
        if stage == "A":
            self.w_in = dt("w_in", [2, D, 2 * D])
            self.b_in = dt("b_in", [2, 2 * D])
            self.b_inuT = dt("b_inuT", [2, 128, KC])
            self.lnv_gT = dt("lnv_gT", [2, 128, KC])
            self.lnv_bT = dt("lnv_bT", [2, 128, KC])
            self.w_s = dt("w_s", [2, 16, 128, 128])
            self.b_s = dt("b_s", [2, 16 * 128])
            self.w_o = dt("w_o", [2, D, D])
            self.kv_ada_w = dt("kv_ada_w", [D, 2 * D])
            self.kv_ada_bT = dt("kv_ada_bT", [128, 32])
            self.w_kv = dt("w_kv", [D, 2 * D])
            for p in range(npass):
                self.io[p]["kt_o"] = dt(f"kt_o{p}", [16, 128, TOK], BF16, "ExternalOutput")
                self.io[p]["v_o"] = dt(f"v_o{p}", [TOK, D], BF16, "ExternalOutput")
                self.io[p]["km_o"] = dt(f"km_o{p}", [128, 16, 4], F32, "ExternalOutput")
        else:
            self.w_q = dt("w_q", [2, D, D])
            self.w_ao = dt("w_ao", [2, D, D])
            for p in range(npass):
                self.io[p]["kt_all"] = dt(f"kt_all{p}", [16, 128, 4096], BF16)
                self.io[p]["v_all"] = dt(f"v_all{p}", [16, 128, 32, 128], BF16)
                self.io[p]["kt_own"] = dt(f"kt_own{p}", [16, 128, TOK], BF16)
                self.io[p]["v_own"] = dt(f"v_own{p}", [16, 128, NT, 128], BF16)
                self.io[p]["km_all"] = dt(f"km_all{p}", [128, 16, 16])
                self.io[p]["pastm"] = dt(f"pastm{p}", [128, NT, 16])
        for p in range(npass):
            self.io[p]["y_d"] = dt(f"y{p}", [TOK, D], F32, "ExternalOutput")

        sb = nc.alloc_sbuf_tensor
        self.X = sb("X", [128, NT, D], F32)
        self.WR = sb("WR", [128, NSLOT, 8192], BF16)
        self.A0 = sb("A0", [128, 16384], BF16)
        self.ident = sb("ident", [128, 128], F32)
        self.ones_bf = sb("ones_bf", [128, 128], BF16)
        self.cact = sb("cact", [128, KC], F32)
        self.cact_bf = sb("cact_bf", [128, KC], BF16)
        self.cact_rep = sb("cact_rep", [128, KC, 128], BF16)
        self.modP = sb("modP", [128, 4, KC], F32)
        self.adabT = sb("adabT", [128, 96], F32)
        self.small = sb("small", [128, 64], F32)
        ARN = 14700
        self.arena = Arena(sb("ARENA", [128, ARN], F32), ARN)
        self.ps = [nc.alloc_psum_tensor(f"ps{i}", [128, 512], F32) for i in range(8)]
        self.ring = Ring(self.fw, self.WR)
        self.out_toks = []

    def PK(self, i):
        return ("ps", i)

    def HT(self):
        return self.A0[:, :].rearrange("p (k t) -> p k t", k=KC)

    def set_pass(self, p):
        for k, v in self.io[p].items():
            setattr(self, k, v)

    def init_consts(self, first=True):
        nc, fw = self.nc, self.fw
        if first:
            fw.op("pool", lambda: nc.gpsimd.memset(self.ident[:], 1.0), W=["ident"])
            fw.op("pool", lambda: nc.gpsimd.affine_select(self.ident[:], self.ident[:], pattern=[[-1, 128]],
                                                          compare_op=ALU.is_equal, fill=0.0, base=0,
                                                          channel_multiplier=1), R=["ident"], W=["ident"])
            fw.op("pool", lambda: nc.gpsimd.memset(self.ones_bf[:], 1.0), W=["ones"])
        fw.dma("sp", self.cact[:], self.cl_d, "c", W=["cact"])
        fw.op("act", lambda: nc.scalar.activation(self.cact[:], self.cact[:], AF.Silu), R=["cact"], W=["cact"])
        fw.op("dve", lambda: nc.vector.tensor_copy(self.cact_bf[:], self.cact[:]), R=["cact"], W=["cactbf"])
        for kc in range(KC):
            fw.op("dve", lambda kc=kc: nc.vector.tensor_scalar(self.cact_rep[:, kc, :], self.ones_bf[:],
                                                               self.cact[:, kc:kc + 1], None, op0=ALU.mult),
                  R=["cact", "ones"], W=["cactrep"])
        for t in range(NT):
            fw.dma("sp", self.X[:, t, :], self.x_d[t * 128:(t + 1) * 128, :], f"x{t}", W=[("X", t)])

    def ada_piece_loader(self, w3, col0, chan_tag):
        src = w3.rearrange("(k p) f -> p k f", p=128)[:, :, col0:col0 + 512]
        def loader(slot, s):
            dst = slot.rearrange("p (k f) -> p k f", k=KC)
            self.fw.dma("pool", dst, src, f"wr{s}", W=[("WR", s)])
        return loader

    def modsP(self, wmat, vecs, bT, dst_cols):
        nc, fw = self.nc, self.fw
        for (vi, add1), dst in zip(vecs, dst_cols):
            bank = self.ps[6]
            for n in range(4):
                slot, wk = self.ring.next(("adaP", vi, n), self.ada_piece_loader(wmat, vi * D + n * 512, "a"))
                w = slot.rearrange("p (k f) -> p k f", k=KC)
                for c in range(4):
                    col = n * 4 + c
                    for kc in range(KC):
                        fw.mm1(bank[:, col:col + 1], w[:, kc, c * 128:(c + 1) * 128], self.cact_bf[:, kc:kc + 1],
                               start=(kc == 0), stop=(kc == KC - 1), R=[wk, "cactbf"], W=[self.PK(6)],
                               last=(kc == KC - 1 and c == 3))
            fw.op("dve", lambda dst=dst, vi=vi: nc.vector.tensor_tensor(dst, bank[:, 0:KC], bT[:, vi * KC:(vi + 1) * KC], op=ALU.add),
                  R=[self.PK(6), "adabT"], W=["modP"])
            if add1:
                fw.op("dve", lambda dst=dst: nc.vector.tensor_scalar(dst, dst, 1.0, None, op0=ALU.add), R=["modP"], W=["modP"])

    def modsBC(self, l, vi, dst, tmp2):
        nc, fw = self.nc, self.fw
        for n in range(4):
            slot, wk = self.ring.next(("adaB", l, vi, n), self.ada_piece_loader(self.ada_w[l], vi * D + n * 512, "a"))
            w = slot.rearrange("p (k f) -> p k f", k=KC)
            tb = tmp2[n % 2]
            fw.dma("sp", tb, self.ada_b[l, vi * D + n * 512: vi * D + (n + 1) * 512].partition_broadcast(128),
                   f"adab{n % 2}", W=[("adab", n % 2)])
            bk = 4 + (n % 2)
            fw.mm(self.ps[bk][:, :], [(self.cact_rep[:, kc, :], w[:, kc, :]) for kc in range(KC)],
                  R=[wk, "cactrep"], W=[self.PK(bk)])
            fw.op("dve", lambda n=n, tb=tb, bk=bk: nc.vector.scalar_tensor_tensor(dst[:, n * 512:(n + 1) * 512], self.ps[bk][:, :], 1.0, tb,
                                                                                op0=ALU.add, op1=ALU.add),
                  R=[self.PK(bk), ("adab", n % 2)], W=[("gbc", n)])

    def build_ht(self, tiles, sc, bi, dst, ntg, router=None):
        nc, fw = self.nc, self.fw
        k = 0
        for kc in range(KC):
            for tg in range(ntg):
                bk = 6 + (k % 2)
                k += 1
                bank = self.ps[bk]
                for i in range(4):
                    t = tiles[tg * 4 + i]
                    fw.tr(bank[:, i * 128:(i + 1) * 128], self.X[:, t, kc * 128:(kc + 1) * 128], self.ident[:],
                          R=[("X", t), "ident"], W=[self.PK(bk)], last=(i == 3))
                fw.op("act", lambda kc=kc, tg=tg, bank=bank: nc.scalar.activation(
                    dst[:, kc, tg * 512:(tg + 1) * 512], bank[:, :], AF.Identity,
                    bias=bi[:, kc:kc + 1], scale=sc[:, kc:kc + 1]),
                    R=[self.PK(bk), "modP"], W=[("HT", kc, tg)])
                if router is not None:
                    hf, wr32, lt = router
                    hb = hf[(kc * ntg + tg) % 2]
                    hk = ("hf", (kc * ntg + tg) % 2)
                    fw.op("dve", lambda kc=kc, bank=bank, hb=hb: nc.vector.tensor_scalar(
                        hb, bank[:, :], sc[:, kc:kc + 1], bi[:, kc:kc + 1], op0=ALU.mult, op1=ALU.add),
                        R=[self.PK(bk), "modP"], W=[hk])
                    fw.mm(self.ps[4 + tg][:, :], [(wr32[:, kc, :], hb)], R=[hk, "wr32"], W=[self.PK(4 + tg)])
                    lts = lt[0:NE, tg * 512:(tg + 1) * 512]
                    if kc == 0:
                        fw.op("dve", lambda tg=tg, lts=lts: nc.vector.tensor_copy(lts, self.ps[4 + tg][0:NE, :]), R=[self.PK(4 + tg)], W=[("LTs", tg)])
                    else:
                        fw.op("dve", lambda tg=tg, lts=lts: nc.vector.tensor_tensor(lts, lts, self.ps[4 + tg][0:NE, :], op=ALU.add),
                              R=[self.PK(4 + tg), ("LTs", tg)], W=[("LTs", tg)])

    def layer_norm(self, tiles, g_ap, b_ap, gbc, bbc, st, mv):
        nc, fw = self.nc, self.fw
        fw.dma("sp", gbc, g_ap.partition_broadcast(128), "lng", W=["lng"])
        fw.dma("sp", bbc, b_ap.partition_broadcast(128), "lnb", W=["lnb"])
        for t in tiles:
            xt = self.X[:, t, :]
            xk = ("X", t)
            for c in range(4):
                fw.op("dve", lambda c=c, xt=xt: nc.vector.bn_stats(st[:, c, :], xt[:, c * 512:(c + 1) * 512]), R=[xk], W=[("st", c)])
            fw.op("dve", lambda: nc.vector.bn_aggr(mv[:, 0:2], st[:, :, :]), R=[("st", c) for c in range(4)], W=["mv"])
            fw.op("dve", lambda: nc.vector.tensor_scalar(mv[:, 2:3], mv[:, 1:2], EPS, None, op0=ALU.add), R=["mv"], W=["mv2"])
            fw.op("act", lambda: nc.scalar.activation(mv[:, 2:3], mv[:, 2:3], AF.Sqrt), R=["mv2"], W=["mv2"])
            fw.op("dve", lambda: nc.vector.reciprocal(mv[:, 2:3], mv[:, 2:3]), R=["mv2"], W=["mv2"])
            fw.op("dve", lambda: nc.vector.scalar_tensor_tensor(mv[:, 3:4], mv[:, 0:1], -1.0, mv[:, 2:3], op0=ALU.mult, op1=ALU.mult),
                  R=["mv", "mv2"], W=["mv3"])
            fw.op("act", lambda xt=xt: nc.scalar.activation(xt, xt, AF.Identity, bias=mv[:, 3:4], scale=mv[:, 2:3]),
                  R=[xk, "mv2", "mv3"], W=[xk])
            fw.op("dve", lambda xt=xt: nc.vector.tensor_tensor(xt, xt, gbc, op=ALU.mult), R=[xk, "lng"], W=[xk])
            fw.op("dve", lambda xt=xt: nc.vector.tensor_tensor(xt, xt, bbc, op=ALU.add), R=[xk, "lnb"], W=[xk])

    def resid_evac(self, bk, t, n, gbc, tmp, tk):
        nc, fw = self.nc, self.fw
        xs = self.X[:, t, n * 512:(n + 1) * 512]
        fw.op("dve", lambda: nc.vector.tensor_tensor(tmp, self.ps[bk][:, :], gbc[:, n * 512:(n + 1) * 512], op=ALU.mult),
              R=[self.PK(bk), ("gbc", n)], W=[tk])
        fw.op("dve", lambda: nc.vector.scalar_tensor_tensor(xs, xs, ALPHA, tmp, op0=ALU.mult, op1=ALU.add),
              R=[("X", t), tk], W=[("X", t)])

    def gmlp(self, li):
        nc, fw, ar = self.nc, self.fw, self.arena
        fw.barrier()
        ar.off = 0
        st = ar.f32(24).rearrange("p (c s) -> p c s", c=4)
        mv = ar.f32(4)
        mark = ar.off
        VB = ar.bf16(4 * D).rearrange("p (t f) -> p t f", t=4)
        g1bc = ar.f32(D)
        Bt = ar.f32(D).rearrange("p (g t) -> p g t", g=16)
        WST = ar.bf16(D).rearrange("p (g t) -> p g t", g=16)
        binv = ar.bf16(D)
        adab = [ar.f32(512), ar.f32(512)]
        tmp = [ar.f32(512), ar.f32(512)]
        binu = ar.f32(KC)
        lnvg = ar.f32(KC)
        lnvb = ar.f32(KC)
        a0f = self.A0[:, :].bitcast(F32)
        wss = a0f[:, 0:2048].rearrange("p (g s) -> p g s", g=16)
        bsbc = a0f[:, 2048:4096].rearrange("p (g t) -> p g t", g=16)
        fw.dma("sp", wss, self.w_s[li].rearrange("g t s -> t g s"), "wss", W=["wss"])
        fw.dma("sp", bsbc, self.b_s[li].partition_broadcast(128), "bsbc", W=["bsbc"])
        fw.dma("pool", binv[0:1, :], self.b_in[li:li + 1, D:2 * D], "binv", W=["binv"])
        fw.dma("sp", binu, self.b_inuT[li], "binu", W=["binu"])
        fw.dma("sp", lnvg, self.lnv_gT[li], "lnvg", W=["lnvg"])
        fw.dma("sp", lnvb, self.lnv_bT[li], "lnvb", W=["lnvb"])
        fw.dma("sp", self.adabT[:], self.ada_bT[li], "adabT", W=["adabT"])
        fw.op("pool", lambda: nc.gpsimd.affine_select(wss, wss, pattern=[[0, 16], [-1, 128]], compare_op=ALU.is_ge,
                                                      fill=0.0, base=0, channel_multiplier=1), R=["wss"], W=["wss"])
        for gq in range(4):
            bk = 6 + gq % 2
            for i in range(4):
                g = gq * 4 + i
                fw.tr(self.ps[bk][:, i * 128:(i + 1) * 128], wss[:, g, :], self.ident[:], R=["wss", "ident"], W=[self.PK(bk)], last=(i == 3))
            fw.op("act", lambda gq=gq, bk=bk: nc.scalar.copy(WST[:, gq * 4:(gq + 1) * 4, :], self.ps[bk][:, :].rearrange("p (g t) -> p g t", g=4)),
                  R=[self.PK(bk)], W=[("WST", gq)])
        for gq in range(4):
            bk = 6 + gq % 2
            fw.mm(self.ps[bk][:, :], [(self.ones_bf[:], WST[:, gq * 4:(gq + 1) * 4, :])], R=[("WST", gq), "ones"], W=[self.PK(bk)])
            for i in range(4):
                g = gq * 4 + i
                fw.op("dve", lambda g=g, i=i, bk=bk: nc.vector.scalar_tensor_tensor(
                    Bt[:, g, :], self.ps[bk][:, i * 128:(i + 1) * 128], lnvb[:, g:g + 1], bsbc[:, g, :], op0=ALU.mult, op1=ALU.add),
                    R=[self.PK(bk), "lnvb", "bsbc"], W=[("Bt", g)])
        self.modsP(self.ada_w[li], [(0, False), (1, True)], self.adabT, [self.modP[:, 0, :], self.modP[:, 1, :]])
        self.modsBC(li, 2, g1bc, adab)
        fw.barrier()
        HTh = self.A0[:, 0:8192].rearrange("p (k t) -> p k t", k=KC)
        UT = self.A0[:, 8192:16384].rearrange("p (k t) -> p k t", k=KC)
        w_in3 = self.w_in[li].rearrange("(k p) f -> p k f", p=128)
        w_o3 = self.w_o[li].rearrange("(k p) f -> p k f", p=128)
        for half in range(2):
            tiles = [half * 4 + i for i in range(4)]
            self.build_ht(tiles, self.modP[:, 1, :], self.modP[:, 0, :], HTh, 1)
            htk = [("HT", kc, 0) for kc in range(KC)]
            k = 0
            for n in range(4):
                def ld(slot, s, n=n):
                    fw.dma("pool", slot.rearrange("p (k f) -> p k f", k=KC), w_in3[:, :, D + n * 512: D + (n + 1) * 512], f"wr{s}", W=[("WR", s)])
                slot, wk = self.ring.next(("wv", li, half, n), ld)
                w = slot.rearrange("p (k f) -> p k f", k=KC)
                for i in range(4):
                    bk = k % 4
                    k += 1
                    pairs = [(HTh[:, kc, i * 128:(i + 1) * 128], w[:, kc, :]) for kc in range(KC)]
                    pairs.append((self.ones_bf[0:1, :], binv[0:1, n * 512:(n + 1) * 512]))
                    fw.mm(self.ps[bk][:, :], pairs, R=[wk, "ones", "binv"] + htk, W=[self.PK(bk)])
                    fw.op("act", lambda i=i, n=n, bk=bk: nc.scalar.activation(VB[:, i, n * 512:(n + 1) * 512], self.ps[bk][:, :], AF.Gelu_apprx_tanh),
                          R=[self.PK(bk)], W=[("VB", i, n)])
            for i in range(4):
                for n in range(4):
                    fw.op("dve", lambda i=i, n=n: nc.vector.bn_stats(st[:, n, :], VB[:, i, n * 512:(n + 1) * 512]), R=[("VB", i, n)], W=[("st", n)])
                fw.op("dve", lambda: nc.vector.bn_aggr(mv[:, 0:2], st[:, :, :]), R=[("st", c) for c in range(4)], W=["mv"])
                fw.op("dve", lambda: nc.vector.tensor_scalar(mv[:, 2:3], mv[:, 1:2], EPS, None, op0=ALU.add), R=["mv"], W=["mv2"])
                fw.op("act", lambda: nc.scalar.activation(mv[:, 2:3], mv[:, 2:3], AF.Sqrt), R=["mv2"], W=["mv2"])
                fw.op("dve", lambda: nc.vector.reciprocal(mv[:, 2:3], mv[:, 2:3]), R=["mv2"], W=["mv2"])
                fw.op("dve", lambda: nc.vector.scalar_tensor_tensor(mv[:, 3:4], mv[:, 0:1], -1.0, mv[:, 2:3], op0=ALU.mult, op1=ALU.mult),
                      R=["mv", "mv2"], W=["mv3"])
                fw.op("act", lambda i=i: nc.scalar.activation(VB[:, i, :], VB[:, i, :], AF.Identity, bias=mv[:, 3:4], scale=mv[:, 2:3]),
                      R=[("VB", i, n) for n in range(4)] + ["mv2", "mv3"], W=[("VB", i, n) for n in range(4)])
            for q in range(4):
                def ld(slot, s, q=q):
                    dst = slot.rearrange("p (c k f) -> p c k f", c=4, k=KC)
                    for c in range(4):
                        fw.dma("pool", dst[:, c], w_in3[:, :, (q * 4 + c) * 128:(q * 4 + c + 1) * 128], f"wr{s}", W=[("WR", s)])
                slot, wk = self.ring.next(("wu", li, half, q), ld)
                w = slot.rearrange("p (c k f) -> p c k f", c=4, k=KC)
                for c in range(4):
                    ch = q * 4 + c
                    bk = k % 4
                    k += 1
                    fw.mm(self.ps[bk][:, :], [(w[:, c, kc, :], HTh[:, kc, :]) for kc in range(KC)], R=[wk] + htk, W=[self.PK(bk)])
                    fw.op("act", lambda ch=ch, bk=bk: nc.scalar.activation(UT[:, ch, :], self.ps[bk][:, :], AF.Gelu_apprx_tanh, bias=binu[:, ch:ch + 1]),
                          R=[self.PK(bk), "binu"], W=[("UT", ch)])
            for i in range(4):
                for gq in range(4):
                    bk = k % 4
                    k += 1
                    for j in range(4):
                        g = gq * 4 + j
                        fw.mm1(self.ps[bk][:, j * 128:(j + 1) * 128], VB[:, i, g * 128:(g + 1) * 128], WST[:, g, :], start=True, stop=True,
                               R=[("VB", i, g // 4), ("WST", gq)], W=[self.PK(bk)], last=(j == 3))
                    tb = tmp[k % 2]
                    tk = ("tmp", k % 2)
                    for j in range(4):
                        g = gq * 4 + j
                        fw.op("dve", lambda g=g, j=j, bk=bk, tb=tb: nc.vector.scalar_tensor_tensor(
                            tb[:, j * 128:(j + 1) * 128], self.ps[bk][:, j * 128:(j + 1) * 128], lnvg[:, g:g + 1], Bt[:, g, :], op0=ALU.mult, op1=ALU.add),
                            R=[self.PK(bk), "lnvg", ("Bt", g)], W=[tk])
                    uview = UT[:, gq * 4:(gq + 1) * 4, i * 128:(i + 1) * 128]
                    fw.op("dve", lambda uview=uview, tb=tb: nc.vector.tensor_tensor(uview, tb.rearrange("p (g t) -> p g t", g=4), uview, op=ALU.mult),
                          R=[tk] + [("UT", gq * 4 + j) for j in range(4)], W=[("UT", gq * 4 + j) for j in range(4)])
            utk = [("UT", c) for c in range(KC)]
            for n in range(4):
                def ld(slot, s, n=n):
                    fw.dma("pool", slot.rearrange("p (k f) -> p k f", k=KC), w_o3[:, :, n * 512:(n + 1) * 512], f"wr{s}", W=[("WR", s)])
                slot, wk = self.ring.next(("wo", li, half, n), ld)
                w = slot.rearrange("p (k f) -> p k f", k=KC)
                for i in range(4):
                    bk = k % 4
                    k += 1
                    fw.mm(self.ps[bk][:, :], [(UT[:, kc, i * 128:(i + 1) * 128], w[:, kc, :]) for kc in range(KC)], R=[wk] + utk, W=[self.PK(bk)])
                    self.resid_evac(bk, tiles[i], n, g1bc, tmp[k % 2], ("tmp", k % 2))
        fw.barrier()
        ar.off = mark
        gbc = ar.f32(D)
        bbc = ar.f32(D)
        self.layer_norm(range(NT), self.ln_g[li, 0], self.ln_b[li, 0], gbc, bbc, st, mv)

    def moe(self, li):
        nc, fw, ar = self.nc, self.fw, self.arena
        fw.barrier()
        ar.off = 0
        g2bc = ar.f32(D)
        DT = [ar.f32(512), ar.f32(512)]
        G = ar.f32(NT * NE).rearrange("p (t e) -> p t e", t=NT)
        Gp = ar.f32(NT * NE).rearrange("p (t e) -> p t e", t=NT)
        bgu = ar.f32(NE * 8).rearrange("p (e c) -> p e c", e=NE)
        st = ar.f32(24).rearrange("p (c s) -> p c s", c=4)
        mv = ar.f32(4)
        mark = ar.off
        ACTT = [ar.bf16(4096).rearrange("p (j t) -> p j t", j=4) for _ in range(2)]
        TT = [ar.f32(512), ar.f32(512)]
        UU = [ar.f32(512), ar.f32(512)]
        g2bf = ar.bf16(D)
        ar.off = mark
        hf = [ar.f32(512), ar.f32(512)]
        wr32 = ar.f32(KC * 128).rearrange("p (k e) -> p k e", k=KC)
        bd32 = ar.f32(D)
        LTs = ar.f32(TOK)
        L = ar.f32(NT * NE).rearrange("p (t e) -> p t e", t=NT)
        MS = ar.f32(NT * NE).rearrange("p (t e) -> p t e", t=NT)
        m8 = ar.f32(8)
        sm = ar.f32(4)
        GT = ar.f32(TOK).rearrange("p (t k) -> p t k", t=NT)
        adab = [ar.f32(512), ar.f32(512)]
        brt = ar.f32(1)
        fw.dma("sp", self.adabT[:], self.ada_bT[li], "adabT", W=["adabT"])
        fw.op("pool", lambda: nc.gpsimd.memset(wr32, 0.0), W=["wr32"])
        fw.dma("sp", wr32[:, :, 0:NE], self.w_r[li].rearrange("(k p) e -> p k e", p=128), "wr32", R=["wr32"], W=["wr32"])
        fw.dma("sp", bd32[0:NE, :], self.b_dn[li], "bd32", W=["bd32"])
        fw.dma("sp", brt[0:NE, :], self.b_rT[li], "brt", W=["brt"])
        fw.dma("sp", bgu, self.b_guT[li], "bgu", W=["bgu"])
        import os
        PRO = int(os.environ.get("MOE_PRO", 99))
        if PRO <= 0:
            return
        fw.op("dve", lambda: nc.vector.tensor_scalar(bgu[:, :, 0:4], bgu[:, :, 0:4], SW_A, None, op0=ALU.mult), R=["bgu"], W=["bgu"])
        fw.op("dve", lambda: nc.vector.tensor_scalar(bgu[:, :, 4:8], bgu[:, :, 4:8], 1.0, None, op0=ALU.add), R=["bgu"], W=["bgu"])
        self.modsP(self.ada_w[li], [(3, False), (4, True)], self.adabT, [self.modP[:, 2, :], self.modP[:, 3, :]])
        if PRO <= 1:
            return
        self.modsBC(li, 5, g2bc, adab)
        HT = self.HT()
        if PRO <= 2:
            return
        self.build_ht(list(range(NT)), self.modP[:, 3, :], self.modP[:, 2, :], HT, 2, router=(hf, wr32, LTs))
        if PRO <= 3:
            return
        for tg in range(2):
            fw.op("act", lambda tg=tg: nc.scalar.activation(LTs[0:NE, tg * 512:(tg + 1) * 512], LTs[0:NE, tg * 512:(tg + 1) * 512], AF.Identity, bias=brt[0:NE, 0:1]),
                  R=[("LTs", tg), "brt"], W=[("LTs", tg)])
        for t in range(NT):
            fw.tr(self.ps[6][:, t * NE:(t + 1) * NE], LTs[0:NE, t * 128:(t + 1) * 128], self.ident[0:NE, 0:NE],
                  R=[("LTs", t // 4), "ident"], W=[self.PK(6)], last=(t == NT - 1))
        fw.op("dve", lambda: nc.vector.tensor_copy(L, self.ps[6][:, 0:NT * NE].rearrange("p (t e) -> p t e", t=NT)), R=[self.PK(6)], W=["L"])
        if PRO <= 4:
            return
        for t in range(NT):
            Lt, Mt, Gt, Gpt = L[:, t, :], MS[:, t, :], G[:, t, :], Gp[:, t, :]
            fw.op("dve", lambda Lt=Lt: nc.vector.max(m8, Lt), R=["L"], W=["m8"])
            fw.op("dve", lambda Lt=Lt, Mt=Mt: nc.vector.tensor_scalar(Mt, Lt, m8[:, 3:4], None, op0=ALU.is_ge), R=["L", "m8"], W=["MS"])
            fw.op("dve", lambda: nc.vector.tensor_scalar(sm[:, 0:1], m8[:, 0:1], -1.0, None, op0=ALU.mult), R=["m8"], W=["sm0"])
            fw.op("act", lambda Lt=Lt, Gt=Gt: nc.scalar.activation(Gt, Lt, AF.Exp, bias=sm[:, 0:1], scale=1.0), R=["L", "sm0"], W=["G"])
            fw.op("dve", lambda Gt=Gt, Mt=Mt: nc.vector.tensor_tensor(Gt, Gt, Mt, op=ALU.mult), R=["G", "MS"], W=["G"])
            fw.op("dve", lambda Gt=Gt: nc.vector.reduce_sum(sm[:, 1:2], Gt, axis=AX.X), R=["G"], W=["sm1"])
            fw.op("dve", lambda: nc.vector.reciprocal(sm[:, 2:3], sm[:, 1:2]), R=["sm1"], W=["sm2"])
            fw.op("dve", lambda Gt=Gt: nc.vector.tensor_scalar(Gt, Gt, sm[:, 2:3], None, op0=ALU.mult), R=["G", "sm2"], W=["G"])
            fw.op("dve", lambda Gt=Gt, Gpt=Gpt: nc.vector.tensor_scalar(Gpt, Gt, 1.0 / SW_A, None, op0=ALU.mult), R=["G"], W=["Gp"])
        if PRO <= 5:
            return
        for t in range(NT):
            bk = 6 + (t // 4)
            fw.tr(self.ps[bk][0:NE, (t % 4) * 128:(t % 4 + 1) * 128], G[:, t, :], self.ident[:], R=["G", "ident"], W=[self.PK(bk)], last=(t % 4 == 3))
        for h in range(2):
            fw.op("act", lambda h=h: nc.scalar.copy(GT[0:NE, h * 4:(h + 1) * 4, :], self.ps[6 + h][0:NE, :].rearrange("p (t k) -> p t k", t=4)),
                  R=[self.PK(6 + h)], W=[("GT", h)])
        if PRO <= 6:
            return
        k = 0
        for t in range(NT):
            for n in range(4):
                bk = 4 + k % 2
                fw.mm(self.ps[bk][:, :], [(GT[0:NE, t, :], bd32[0:NE, n * 512:(n + 1) * 512])], R=[("GT", t // 4), "bd32"], W=[self.PK(bk)])
                self.resid_evac(bk, t, n, g2bc, DT[k % 2], ("DT", k % 2))
                k += 1
        fw.barrier()
        fw.op("dve", lambda: nc.vector.tensor_copy(g2bf, g2bc), R=[("gbc", n) for n in range(4)], W=["g2bf"])
        w_gu = self.w_gu[li]
        w_dn = self.w_dn[li]
        htk = [[("HT", kc, th) for kc in range(KC)] for th in range(2)]

        def gu_unit(e, j, state):
            def ld(slot, s, e=e, j=j):
                dst = slot[:, 0:4096].rearrange("p (h k f) -> p h k f", h=2, k=KC)
                src = w_gu[e].rearrange("(k p) f -> p k f", p=128)
                for h in range(2):
                    c0 = h * 512 + j * 128
                    fw.dma("pool", dst[:, h], src[:, :, c0:c0 + 128], f"wr{s}", W=[("WR", s)])
            slot, wk = self.ring.next(("gu", li, e, j), ld)
            w4 = slot[:, 0:4096].rearrange("p (h k f) -> p h k f", h=2, k=KC)
            eb = e % 2
            for th in range(2):
                fw.mm(self.ps[th * 2][:, :], [(w4[:, 0, kc, :], HT[:, kc, th * 512:(th + 1) * 512]) for kc in range(KC)],
                      R=[wk] + htk[th], W=[self.PK(th * 2)])
                fw.mm(self.ps[th * 2 + 1][:, :], [(w4[:, 1, kc, :], HT[:, kc, th * 512:(th + 1) * 512]) for kc in range(KC)],
                      R=[wk] + htk[th], W=[self.PK(th * 2 + 1)])
                fw.op("act", lambda th=th: nc.scalar.activation(TT[th], self.ps[th * 2][:, :], AF.Silu, bias=bgu[:, e, j:j + 1], scale=SW_A),
                      R=[self.PK(th * 2), "bgu"], W=[("TT", th)])
                fw.op("act", lambda th=th: nc.scalar.activation(UU[th], self.ps[th * 2 + 1][:, :], AF.Identity, bias=bgu[:, e, 4 + j:5 + j]),
                      R=[self.PK(th * 2 + 1), "bgu"], W=[("UU", th)])
                fw.op("dve", lambda th=th: nc.vector.tensor_scalar(UU[th], UU[th], 8.0, -6.0, op0=ALU.min, op1=ALU.max), R=[("UU", th)], W=[("UU", th)])
                fw.op("dve", lambda th=th: nc.vector.scalar_tensor_tensor(ACTT[eb][:, j, th * 512:(th + 1) * 512], TT[th], C7, UU[th], op0=ALU.min, op1=ALU.mult),
                      R=[("TT", th), ("UU", th)], W=[("ACTT", eb, j)])

        def down_unit(e, state):
            def ld(slot, s, e=e):
                fw.dma("pool", slot.rearrange("p (j n) -> p j n", j=4), w_dn[e].rearrange("(j p) n -> p j n", p=128), f"wr{s}", W=[("WR", s)])
            slot, wk = self.ring.next(("dn", li, e), ld)
            w = slot.rearrange("p (j n) -> p j n", j=4)
            eb = e % 2
            k = state.get("dk", 0)
            for j in range(4):
                fw.op("dve", lambda j=j: nc.vector.tensor_tensor(w[:, j, :], w[:, j, :], g2bf, op=ALU.mult), R=[wk, "g2bf"], W=[wk])
            for t in range(NT):
                for n in range(4):
                    bk = 4 + k % 4
                    k += 1
                    fw.mm(self.ps[bk][:, :], [(ACTT[eb][:, j, t * 128:(t + 1) * 128], w[:, j, n * 512:(n + 1) * 512]) for j in range(4)],
                          R=[wk] + [("ACTT", eb, j) for j in range(4)], W=[self.PK(bk)])
                    xs = self.X[:, t, n * 512:(n + 1) * 512]
                    fw.op("dve", lambda bk=bk, xs=xs, t=t: nc.vector.scalar_tensor_tensor(
                        xs, self.ps[bk][:, :], Gp[:, t, e:e + 1], xs, op0=ALU.mult, op1=ALU.add),
                        R=[self.PK(bk), "Gp", ("X", t)], W=[("X", t)])
            state["dk"] = k

        state = {}
        import os
        nexp = int(os.environ.get("MOE_NEXP", NE))
        for e in range(nexp):
            gu_unit(e, 0, state)
            if e > 0:
                down_unit(e - 1, state)
            for j in range(1, 4):
                gu_unit(e, j, state)
        if nexp > 0:
            down_unit(nexp - 1, state)
        fw.barrier()
        ar.off = mark
        gbc = ar.f32(D)
        bbc = ar.f32(D)
        self.layer_norm(range(NT), self.ln_g[li, 1], self.ln_b[li, 1], gbc, bbc, st, mv)

    def kv(self):
        nc, fw, ar = self.nc, self.fw, self.arena
        fw.barrier()
        ar.off = 0
        KTs = [ar.bf16(TOK), ar.bf16(TOK)]
        Vs = [ar.bf16(512), ar.bf16(512)]
        kms = ar.f32(64).rearrange("p (h b) -> p h b", h=16)
        kvb = ar.f32(32)
        modKV = ar.f32(32).rearrange("p (v k) -> p v k", v=2)
        fw.dma("sp", kvb, self.kv_ada_bT, "kvb", W=["adabT"])
        self.modsP(self.kv_ada_w, [(0, False), (1, True)], kvb, [modKV[:, 0, :], modKV[:, 1, :]])
        HT = self.HT()
        self.build_ht(list(range(NT)), modKV[:, 1, :], modKV[:, 0, :], HT, 2)
        htk = [[("HT", kc, th) for kc in range(KC)] for th in range(2)]
        w3 = self.w_kv.rearrange("(k p) f -> p k f", p=128)
        k = 0
        for q in range(4):
            def ld(slot, s, q=q):
                dst = slot.rearrange("p (c k f) -> p c k f", c=4, k=KC)
                for c in range(4):
                    fw.dma("pool", dst[:, c], w3[:, :, (q * 4 + c) * 128:(q * 4 + c + 1) * 128], f"wr{s}", W=[("WR", s)])
            slot, wk = self.ring.next(("wk", q), ld)
            w = slot.rearrange("p (c k f) -> p c k f", c=4, k=KC)
            for c in range(4):
                h = q * 4 + c
                kb = KTs[h % 2]
                kk = ("KTs", h % 2)
                for th in range(2):
                    bk = k % 4
                    k += 1
                    fw.mm(self.ps[bk][:, :], [(w[:, c, kc, :], HT[:, kc, th * 512:(th + 1) * 512]) for kc in range(KC)], R=[wk] + htk[th], W=[self.PK(bk)])
                    fw.op("act", lambda kb=kb, th=th, bk=bk: nc.scalar.copy(kb[:, th * 512:(th + 1) * 512], self.ps[bk][:, :]), R=[self.PK(bk)], W=[kk])
                    fw.op("dve", lambda h=h, th=th, bk=bk: nc.vector.tensor_reduce(kms[:, h, th * 2:(th + 1) * 2], self.ps[bk][:, :].rearrange("p (b s) -> p b s", b=2),
                                                                                   op=ALU.add, axis=AX.X), R=[self.PK(bk)], W=["kms"])
                self.out_toks.append(fw.dma("sp", self.kt_o[h], kb, f"kto{h % 2}", R=[kk]))
        fw.op("dve", lambda: nc.vector.tensor_scalar(kms, kms, 1.0 / 256.0, None, op0=ALU.mult), R=["kms"], W=["kms"])
        self.out_toks.append(fw.dma("sp", self.km_o, kms, "kmo", R=["kms"]))
        k2 = 0
        for n in range(4):
            def ld(slot, s, n=n):
                fw.dma("pool", slot.rearrange("p (k f) -> p k f", k=KC), w3[:, :, D + n * 512: D + (n + 1) * 512], f"wr{s}", W=[("WR", s)])
            slot, wk = self.ring.next(("wvv", n), ld)
            w = slot.rearrange("p (k f) -> p k f", k=KC)
            for t in range(NT):
                bk = k % 4
                k += 1
                vb = Vs[k2 % 2]
                vk = ("Vs", k2 % 2)
                k2 += 1
                fw.mm(self.ps[bk][:, :], [(HT[:, kc, t * 128:(t + 1) * 128], w[:, kc, :]) for kc in range(KC)], R=[wk] + htk[t // 4], W=[self.PK(bk)])
                fw.op("act", lambda vb=vb, bk=bk: nc.scalar.copy(vb, self.ps[bk][:, :]), R=[self.PK(bk)], W=[vk])
                self.out_toks.append(fw.dma("sp", self.v_o[t * 128:(t + 1) * 128, n * 512:(n + 1) * 512], vb, f"vo{k2 % 2}", R=[vk]))

    def attn(self, li):
        nc, fw, ar = self.nc, self.fw, self.arena
        fw.barrier()
        ar.off = 0
        st = ar.f32(24).rearrange("p (c s) -> p c s", c=4)
        mv = ar.f32(4)
        mark = ar.off
        QT = ar.bf16(KC * TOK).rearrange("p (h t) -> p h t", h=16)
        KMb = ar.bf16(256).rearrange("p (h n) -> p h n", h=16)
        PM = ar.f32(128)
        KTo = [ar.bf16(TOK) for _ in range(2)]
        Vo = [ar.bf16(TOK).rearrange("p (t d) -> p t d", t=NT) for _ in range(2)]
        MBT = [ar.bf16(TOK) for _ in range(2)]
        GS = ar.f32(128)
        SEL = ar.f32(128)
        m8 = ar.f32(64).rearrange("p (t e) -> p t e", t=NT)
        thr = ar.f32(8)
        PT = [ar.bf16(256) for _ in range(3)]
        RI = [ar.f32(256) for _ in range(2)]
        EN = ar.bf16(16 * 128).rearrange("p (n k) -> p n k", n=16)
        TRI2 = ar.bf16(256)
        TRI3 = ar.bf16(256)
        identb = ar.bf16(128)
        fw.dma("sp", self.adabT[:], self.ada_bT[li], "adabT", W=["adabT"])
        fw.dma("pool", KMb, self.km_all, "kmb", W=["KMb"])
        fw.op("pool", lambda: nc.gpsimd.memset(EN[0:16], 1.0), W=["EN"])
        fw.op("pool", lambda: nc.gpsimd.affine_select(EN[0:16], EN[0:16], pattern=[[-1, 16], [0, 128]], compare_op=ALU.is_equal,
                                                      fill=0.0, base=0, channel_multiplier=1), R=["EN"], W=["EN"])
        fw.op("pool", lambda: nc.gpsimd.memset(TRI2, 0.0), W=["TRI2"])
        fw.op("pool", lambda: nc.gpsimd.affine_select(TRI2[:, 0:128], TRI2[:, 0:128], pattern=[[1, 128]], compare_op=ALU.is_ge,
                                                      fill=NEG, base=0, channel_multiplier=-1), R=["TRI2"], W=["TRI2"])
        fw.op("pool", lambda: nc.gpsimd.memset(TRI3, NEG), W=["TRI3"])
        fw.op("pool", lambda: nc.gpsimd.tensor_copy(TRI3[:, 128:256], TRI2[:, 0:128]), R=["TRI2", "TRI3"], W=["TRI3"])
        fw.op("dve", lambda: nc.vector.tensor_copy(identb, self.ident[:]), R=["ident"], W=["identb"])
        self.modsP(self.ada_w[li], [(0, False), (1, True)], self.adabT, [self.modP[:, 0, :], self.modP[:, 1, :]])
        HT = self.HT()
        self.build_ht(list(range(NT)), self.modP[:, 1, :], self.modP[:, 0, :], HT, 2)
        htk = [[("HT", kc, th) for kc in range(KC)] for th in range(2)]
        wq3 = self.w_q[li].rearrange("(k p) f -> p k f", p=128)
        k = 0
        for q in range(4):
            def ld(slot, s, q=q):
                dst = slot.rearrange("p (c k f) -> p c k f", c=4, k=KC)
                for c in range(4):
                    fw.dma("pool", dst[:, c], wq3[:, :, (q * 4 + c) * 128:(q * 4 + c + 1) * 128], f"wr{s}", W=[("WR", s)])
            slot, wk = self.ring.next(("wq", li, q), ld)
            w = slot.rearrange("p (c k f) -> p c k f", c=4, k=KC)
            for c in range(4):
                h = q * 4 + c
                for th in range(2):
                    bk = k % 4
                    k += 1
                    fw.mm(self.ps[bk][:, :], [(w[:, c, kc, :], HT[:, kc, th * 512:(th + 1) * 512]) for kc in range(KC)], R=[wk] + htk[th], W=[self.PK(bk)])
                    fw.op("act", lambda h=h, th=th, bk=bk: nc.scalar.activation(QT[:, h, th * 512:(th + 1) * 512], self.ps[bk][:, :], AF.Identity, scale=128.0 ** -0.5),
                          R=[self.PK(bk)], W=[("QT", h)])
        fw.barrier()
        OT = self.HT()
        fw.dma("sp", PM, self.pastm.rearrange("p t n -> p (t n)"), "pm", W=["PM"])
        sk = 0
        for h in range(16):
            hb = h % 2
            def ld(slot, s, h=h, kta=self.kt_all, va=self.v_all):
                fw.dma("pool", slot[:, 0:4096], kta[h], f"wr{s}", W=[("WR", s)])
                fw.dma("pool", slot[:, 4096:8192].rearrange("p (t d) -> p t d", t=32), va[h], f"wr{s}", W=[("WR", s)])
            slot, wk = self.ring.next(("kvh", li, h), ld)
            KTa = slot[:, 0:4096]
            Va = slot[:, 4096:8192].rearrange("p (t d) -> p t d", t=32)
            fw.dma("sp", KTo[hb], self.kt_own[h], f"kto{hb}", W=[("KTo", hb)])
            fw.dma("sp", Vo[hb], self.v_own[h], f"vo{hb}", W=[("Vo", hb)])
            for t in range(NT):
                fw.mm1(self.ps[6][:, t * 16:(t + 1) * 16], QT[:, h, t * 128:(t + 1) * 128], KMb[:, h, :], start=True, stop=True,
                       R=[("QT", h), "KMb"], W=[self.PK(6)], last=(t == NT - 1))
            fw.op("dve", lambda: nc.vector.tensor_tensor(GS, self.ps[6][:, 0:128], PM, op=ALU.add), R=[self.PK(6), "PM"], W=["GS"])
            for t in range(NT):
                fw.op("dve", lambda t=t: nc.vector.max(m8[:, t, :], GS[:, t * 16:(t + 1) * 16]), R=["GS"], W=["m8"])
            fw.op("dve", lambda: nc.vector.tensor_scalar(thr, m8[:, :, 2], -1e29, None, op0=ALU.max), R=["m8"], W=["thr"])
            for t in range(NT):
                fw.op("dve", lambda t=t: nc.vector.tensor_scalar(SEL[:, t * 16:(t + 1) * 16], GS[:, t * 16:(t + 1) * 16], thr[:, t:t + 1], None, op0=ALU.is_ge),
                      R=["GS", "thr"], W=["SEL"])
            fw.op("dve", lambda: nc.vector.tensor_scalar(SEL, SEL, -1.0, -NEG, op0=ALU.add, op1=ALU.mult), R=["SEL"], W=["SEL"])
            for t in range(NT):
                bk = 7
                fw.tr(self.ps[bk][0:16, (t % 4) * 128:(t % 4 + 1) * 128], SEL[:, t * 16:(t + 1) * 16], self.ident[:], R=["SEL", "ident"], W=[self.PK(bk)], last=(t % 4 == 3))
                if t % 4 == 3:
                    tg = t // 4
                    fw.op("act", lambda tg=tg: nc.scalar.copy(MBT[hb][0:16, tg * 512:(tg + 1) * 512], self.ps[7][0:16, :]), R=[self.PK(7)], W=[("MBT", hb, tg)])
            for lb in range(4):
                q0 = lb * 256
                qv = QT[:, h, q0:q0 + 256]
                items = []
                for n in range(4 * lb + 3):
                    for kh in range(2):
                        kt = n * 2 + kh
                        items.append((KTa[:, kt * 128:(kt + 1) * 128], EN[0:16, n, :], MBT[hb][0:16, q0:q0 + 256], Va[:, kt, :], 256, 0,
                                      [wk, ("MBT", hb, lb // 2), "EN"]))
                items.append((KTo[hb][:, q0:q0 + 128], identb, TRI2, Vo[hb][:, 2 * lb, :], 256, 0, [("KTo", hb), ("Vo", hb), "identb", "TRI2"]))
                items.append((KTo[hb][:, q0 + 128:q0 + 256], identb, TRI3, Vo[hb][:, 2 * lb + 1, :], 256, 0, [("KTo", hb), ("Vo", hb), "identb", "TRI3"]))
                ni = len(items)
                for ii, (kl, ml, mr, vt, ncol, c0, keys) in enumerate(items):
                    sb = sk % 2
                    pb = sk % 3
                    sk += 1
                    S = self.ps[sb][:, 0:ncol]
                    fw.mm1(S, kl, qv[:, c0:c0 + ncol], start=True, stop=False, R=keys + [("QT", h)], W=[self.PK(sb)])
                    fw.mm1(S, ml, mr, start=False, stop=True, R=keys, W=[self.PK(sb)], last=True)
                    fw.op("act", lambda S=S, pb=pb, ncol=ncol: nc.scalar.activation(PT[pb][:, 0:ncol], S, AF.Exp), R=[self.PK(sb)], W=[("PT", pb)])
                    fw.mm1(self.ps[2][:, c0:c0 + ncol], vt, PT[pb][:, 0:ncol], start=(ii == 0), stop=(ii == ni - 1), R=keys + [("PT", pb)], W=[self.PK(2)])
                    fw.mm1(self.ps[3][:, c0:c0 + ncol], self.ones_bf[:], PT[pb][:, 0:ncol], start=(ii == 0), stop=(ii == ni - 1), R=["ones", ("PT", pb)], W=[self.PK(3)],
                           last=True)
                rb = lb % 2
                fw.op("dve", lambda rb=rb: nc.vector.reciprocal(RI[rb], self.ps[3][:, 0:256]), R=[self.PK(3)], W=[("RI", rb)])
                fw.op("dve", lambda rb=rb, q0=q0, h=h: nc.vector.tensor_tensor(OT[:, h, q0:q0 + 256], self.ps[2][:, 0:256], RI[rb], op=ALU.mult),
                      R=[self.PK(2), ("RI", rb)], W=[("OT", h)])
        fw.barrier()
        ar.off = mark
        g1bc = ar.f32(D)
        adab = [ar.f32(512), ar.f32(512)]
        tmp = [ar.f32(512), ar.f32(512)]
        self.modsBC(li, 2, g1bc, adab)
        wo3 = self.w_ao[li].rearrange("(k p) f -> p k f", p=128)
        otk = [("OT", h) for h in range(16)]
        k = 0
        for n in range(4):
            def ld(slot, s, n=n):
                fw.dma("pool", slot.rearrange("p (k f) -> p k f", k=KC), wo3[:, :, n * 512:(n + 1) * 512], f"wr{s}", W=[("WR", s)])
            slot, wk = self.ring.next(("wao", li, n), ld)
            w = slot.rearrange("p (k f) -> p k f", k=KC)
            for t in range(NT):
                bk = k % 4
                k += 1
                fw.mm(self.ps[bk][:, :], [(OT[:, hh, t * 128:(t + 1) * 128], w[:, hh, :]) for hh in range(16)], R=[wk] + otk, W=[self.PK(bk)])
                self.resid_evac(bk, t, n, g1bc, tmp[k % 2], ("tmp", k % 2))
        fw.barrier()
        gbc = ar.f32(D)
        bbc = ar.f32(D)
        self.layer_norm(range(NT), self.ln_g[li, 0], self.ln_b[li, 0], gbc, bbc, st, mv)

    def phases(self):
        ph = []
        if self.stage == "A":
            for li in range(2):
                ph.append(lambda li=li: self.gmlp(li))
                ph.append(lambda li=li: self.moe(li))
            ph.append(self.kv)
        else:
            for li in range(2):
                ph.append(lambda li=li: self.attn(li))
                ph.append(lambda li=li: self.moe(li))
        return ph

    def emit(self):
        ph = self.phases()
        if self.stop_after is not None:
            ph = ph[:self.stop_after]
        fw = self.fw
        fw.dry = True
        for p in range(self.npass):
            self.set_pass(p)
            for f in ph:
                f()
        fw.dry = False
        self.ring.reset()
        for p in range(self.npass):
            self.set_pass(p)
            if p > 0:
                fw.barrier()
            self.init_consts(first=(p == 0))
            for f in ph:
                f()
            fw.barrier()
            for t in range(NT):
                self.out_toks.append(fw.dma("sp", self.y_d[t * 128:(t + 1) * 128, :], self.X[:, t, :], f"x{t}", R=[("X", t)]))
        fw.finish(self.out_toks)
        return self.nc


BLOCKS = lambda j: [j, 7 - j, 8 + j, 15 - j]


def _common_maps(inp, layers):
    l0 = layers[0]
    sl = slice(l0, l0 + 2)
    f = lambda a: np.ascontiguousarray(a, dtype=np.float32)
    m = {
        "ada_w": f(inp["ada_w"][sl]), "ada_b": f(inp["ada_b"][sl]),
        "ada_bT": f(inp["ada_b"][sl].reshape(2, 96, 128).transpose(0, 2, 1)),
        "ln_g": f(inp["ln_g"][sl]), "ln_b": f(inp["ln_b"][sl]),
        "w_r": f(inp["moe_w_router"][sl]), "b_rT": f(inp["moe_b_router"][sl].reshape(2, NE, 1)),
        "w_gu": f(inp["moe_w_gu"][sl]),
        "b_guT": f(inp["moe_b_gu"][sl].reshape(2, NE, 8, 128).transpose(0, 3, 1, 2)),
        "w_dn": f(inp["moe_w_down"][sl]), "b_dn": f(inp["moe_b_down"][sl]),
    }
    return m


def _cl(c, b):
    return np.ascontiguousarray(c[b].reshape(KC, 128).T, dtype=np.float32)


_PROG_CACHE = {}


def _get_prog(stage, stop_after=None, npass=1):
    key = (stage, stop_after, npass)
    if key not in _PROG_CACHE:
        p = Prog(stage, stop_after, npass)
        p.emit()
        _PROG_CACHE[key] = p
    return _PROG_CACHE[key]


def _vx(inp, v):
    b, j = v // 4, v % 4
    return np.ascontiguousarray(np.concatenate([inp["x"][b, g * 256:(g + 1) * 256] for g in BLOCKS(j)], axis=0), dtype=np.float32)


def run_stage_a(inp, stop_after=None, groups=None):
    f = lambda a: np.ascontiguousarray(a, dtype=np.float32)
    groups = groups or [[v] for v in range(8)]
    npass = len(groups[0])
    com = _common_maps(inp, [0, 1])
    com.update({
        "w_in": f(inp["gm_w_in"]), "b_in": f(inp["gm_b_in"]),
        "b_inuT": f(inp["gm_b_in"][:, :D].reshape(2, KC, 128).transpose(0, 2, 1)),
        "lnv_gT": f(inp["gm_lnv_g"].reshape(2, KC, 128).transpose(0, 2, 1)),
        "lnv_bT": f(inp["gm_lnv_b"].reshape(2, KC, 128).transpose(0, 2, 1)),
        "w_s": f(inp["gm_w_s"]), "b_s": f(inp["gm_b_s"].reshape(2, 16 * 128)),
        "w_o": f(inp["gm_w_out"]), "kv_ada_w": f(inp["kv_ada_w"]),
        "kv_ada_bT": f(inp["kv_ada_b"].reshape(32, 128).T), "w_kv": f(inp["w_kv"]),
    })
    maps = []
    for grp in groups:
        m = dict(com)
        for p, v in enumerate(grp):
            m[f"x{p}"] = _vx(inp, v)
            m[f"cl{p}"] = _cl(inp["c"], v // 4)
        maps.append(m)
    prog = _get_prog("A", stop_after, npass)
    res = run_bass_kernel_spmd(prog.nc, maps, core_ids=list(range(len(maps)))).results
    ra = {}
    for c, grp in enumerate(groups):
        for p, v in enumerate(grp):
            ra[v] = {k: np.asarray(res[c][f"{k}{p}"]) for k in ("y", "kt_o", "v_o", "km_o")}
    return ra


def run_stage_b(inp, ra, stop_after=None, groups=None):
    f = lambda a: np.ascontiguousarray(a, dtype=np.float32)
    groups = groups or [[v] for v in range(8)]
    npass = len(groups[0])
    com = _common_maps(inp, [2, 3])
    com.update({"w_q": f(inp["attn_w_q"]), "w_ao": f(inp["attn_w_out"])})
    bf = ml_dtypes.bfloat16
    per_batch = {}
    for b in sorted(set(v // 4 for grp in groups for v in grp)):
        kt_all = np.zeros((16, 128, 4096), dtype=bf)
        v_tok = np.zeros((4096, D), dtype=bf)
        km_all = np.zeros((128, 16, 16), dtype=np.float32)
        for j in range(4):
            r = ra[b * 4 + j]
            for lb, g in enumerate(BLOCKS(j)):
                kt_all[:, :, g * 256:(g + 1) * 256] = r["kt_o"][:, :, lb * 256:(lb + 1) * 256]
                v_tok[g * 256:(g + 1) * 256] = r["v_o"][lb * 256:(lb + 1) * 256]
                km_all[:, :, g] = r["km_o"][:, :, lb]
        v_all = np.ascontiguousarray(v_tok.reshape(32, 128, 16, 128).transpose(2, 1, 0, 3))
        per_batch[b] = (kt_all, v_all, km_all)
    maps = []
    for grp in groups:
        m = dict(com)
        for p, v in enumerate(grp):
            b, j = v // 4, v % 4
            r = ra[v]
            m[f"x{p}"] = f(r["y"])
            m[f"cl{p}"] = _cl(inp["c"], b)
            m[f"kt_all{p}"], m[f"v_all{p}"], m[f"km_all{p}"] = per_batch[b]
            m[f"kt_own{p}"] = np.ascontiguousarray(r["kt_o"])
            m[f"v_own{p}"] = np.ascontiguousarray(np.asarray(r["v_o"]).reshape(NT, 128, 16, 128).transpose(2, 1, 0, 3))
            pm = np.zeros((128, NT, 16), dtype=np.float32)
            for lb, g in enumerate(BLOCKS(j)):
                pm[:, 2 * lb:2 * lb + 2, g:] = -1e30
            m[f"pastm{p}"] = pm
        maps.append(m)
    prog = _get_prog("B", stop_after, npass)
    res = run_bass_kernel_spmd(prog.nc, maps, core_ids=list(range(len(maps)))).results
    rb = {}
    for c, grp in enumerate(groups):
        for p, v in enumerate(grp):
            rb[v] = {"y": np.asarray(res[c][f"y{p}"])}
    return rb


def kernel(**inp):
    inp = {k: np.asarray(v) for k, v in inp.items()}
    ra = run_stage_a(inp)
    rb = run_stage_b(inp, ra)
    out = np.zeros((2, 4096, D), dtype=np.float32)
    for v in range(8):
        b, j = v // 4, v % 4
        for lb, g in enumerate(BLOCKS(j)):
            out[b, g * 256:(g + 1) * 256] = rb[v]["y"][lb * 256:(lb + 1) * 256]
    return out
```

```python
import math
import numpy as np
import ml_dtypes
import concourse.bass as bass
import concourse.mybir as mybir
from concourse.bass_utils import run_bass_kernel_spmd

F32 = mybir.dt.float32
BF16 = mybir.dt.bfloat16
AF = mybir.ActivationFunctionType
ALU = mybir.AluOpType
AX = mybir.AxisListType

D = 2048
KC = 16
NT = 8
TOK = 1024
NE = 32
ALPHA = 8.0 ** 0.25
EPS = 1e-5
SW_A = 1.702
C7 = SW_A * 7.0 / (1.0 + math.exp(-SW_A * 7.0))
NEG = -30000.0
SEM_CAP = 30000
NSLOT = 3


class Eng:
    def __init__(self, fw, name, handle):
        self.fw, self.name, self.h = fw, name, handle
        self.nsem, self.cnt, self.waited = 0, 0, {}
        self.pend_r, self.pend_w = [], []
        self._newsem()

    def _newsem(self):
        self.sem = self.fw.nc.alloc_semaphore(f"{self.name}_e{self.nsem}")
        self.nsem += 1
        self.cnt = 0

    def wait(self, tok):
        sem, val = tok
        if self.waited.get(sem.name, 0) >= val:
            return
        self.h.wait_ge(sem, val)
        self.waited[sem.name] = val

    def mark(self, ins):
        if self.cnt >= SEM_CAP:
            self._newsem()
        ins.then_inc(self.sem, 1)
        self.cnt += 1
        return (self.sem, self.cnt)


class FW:
    def __init__(self, nc):
        self.nc = nc
        self.dry = False
        self.E = {"pe": Eng(self, "pe", nc.tensor), "act": Eng(self, "act", nc.scalar),
                  "dve": Eng(self, "dve", nc.vector), "pool": Eng(self, "pool", nc.gpsimd),
                  "sp": Eng(self, "sp", nc.sync)}
        self.lw, self.rd, self.dsem = {}, {}, {}
        self.ninst = 0

    @staticmethod
    def _px(R, W):
        Rp = [k for k in R if isinstance(k, tuple) and k[0] == "ps"]
        if Rp:
            R = [k for k in R if not (isinstance(k, tuple) and k[0] == "ps")]
            W = list(W) + Rp
        return list(R), list(W)

    def _deps(self, eng, R, W):
        e = self.E[eng]
        pe = self.E["pe"]
        if eng != "pe" and pe.pend_r:
            for k in W:
                if k in pe.pend_r:
                    raise RuntimeError(f"write to {k} on {eng} while PE read has no token yet")
        for k in R:
            t = self.lw.get(k)
            if t is not None and not (t[1] == "pe" and eng == "pe"):
                e.wait(t[0])
        for k in W:
            t = self.lw.get(k)
            if t is not None and not (t[1] == "pe" and eng == "pe") and t[1] != getattr(self, "_skip_src", None):
                e.wait(t[0])
            for t in self.rd.get(k, ()):
                if not (t[1] == "pe" and eng == "pe"):
                    e.wait(t[0])

    def _commit(self, src, tok, R, W):
        for k in W:
            self.lw[k] = (tok, src)
            self.rd[k] = []
        for k in R:
            lst = self.rd.setdefault(k, [])
            lst[:] = [x for x in lst if x[1] != src]
            lst.append((tok, src))

    def _pe_done(self, ins, R, W):
        e = self.E["pe"]
        tok = e.mark(ins)
        R = list(R) + e.pend_r
        W = list(W) + e.pend_w
        e.pend_r, e.pend_w = [], []
        self._commit("pe", tok, R, W)
        return tok

    def op(self, eng, fn, R=(), W=()):
        if self.dry:
            return None
        R, W = self._px(R, W)
        self._deps(eng, R, W)
        ins = fn()
        self.ninst += 1
        tok = self.E[eng].mark(ins)
        self._commit(eng, tok, R, W)
        return tok

    def mm(self, out, pairs, R=(), W=()):
        if self.dry:
            return None
        self._deps("pe", R, W)
        n = len(pairs)
        ins = None
        for i, (a, b) in enumerate(pairs):
            ins = self.nc.tensor.matmul(out, a, b, start=(i == 0), stop=(i == n - 1))
        self.ninst += n
        return self._pe_done(ins, R, W)

    def mm1(self, out, a, b, start, stop, R=(), W=(), last=False):
        if self.dry:
            return None
        self._deps("pe", R, W)
        ins = self.nc.tensor.matmul(out, a, b, start=start, stop=stop)
        self.ninst += 1
        if last:
            return self._pe_done(ins, R, W)
        e = self.E["pe"]
        e.pend_r += list(R)
        e.pend_w += list(W)
        return None

    def tr(self, out, in_, ident, R=(), W=(), last=True):
        if self.dry:
            return None
        self._deps("pe", R, W)
        ins = self.nc.tensor.transpose(out, in_, ident)
        self.ninst += 1
        if last:
            return self._pe_done(ins, R, W)
        e = self.E["pe"]
        e.pend_r += list(R)
        e.pend_w += list(W)
        return None

    def dma(self, q, out, in_, chan, R=(), W=()):
        if self.dry:
            return None
        self._skip_src = "dma:" + chan
        self._deps(q, R, W)
        self._skip_src = None
        if chan not in self.dsem:
            self.dsem[chan] = [self.nc.alloc_semaphore(f"d_{chan}"), 0]
        ds = self.dsem[chan]
        self.E[q].h.dma_start(out=out, in_=in_).then_inc(ds[0], 16)
        self.ninst += 1
        ds[1] += 16
        tok = (ds[0], ds[1])
        self._commit("dma:" + chan, tok, R, W)
        return tok

    def barrier(self):
        if self.dry:
            return
        best = {}
        def add(tok):
            sem, val = tok
            if best.get(sem.name, (None, 0))[1] < val:
                best[sem.name] = (sem, val)
        for (t, s) in self.lw.values():
            add(t)
        for lst in self.rd.values():
            for (t, s) in lst:
                add(t)
        for en in ("pe", "act", "dve", "pool", "sp"):
            for tok in best.values():
                self.E[en].wait(tok)

    def finish(self, toks):
        for t in toks:
            if t is not None:
                self.E["sp"].wait(t)


class Ring:
    def __init__(self, fw, wr):
        self.fw, self.wr = fw, wr
        self.plan, self.cons, self.issued = [], 0, 0

    def reset(self):
        self.cons, self.issued = 0, 0

    def next(self, tag, loader):
        if self.fw.dry:
            self.plan.append((tag, loader))
            i = len(self.plan) - 1
            return self.wr[:, i % NSLOT, :], ("WR", i % NSLOT)
        i = self.cons
        assert self.plan[i][0] == tag, (self.plan[i][0], tag)
        while self.issued < min(len(self.plan), i + NSLOT):
            k = self.issued
            s = k % NSLOT
            self.plan[k][1](self.wr[:, s, :], s)
            self.issued += 1
        self.cons += 1
        return self.wr[:, i % NSLOT, :], ("WR", i % NSLOT)


class Arena:
    def __init__(self, t, n):
        self.t, self.n, self.off = t, n, 0

    def f32(self, n):
        assert self.off + n <= self.n, ("arena overflow", self.off, n, self.n)
        v = self.t[:, self.off:self.off + n]
        self.off += n
        return v

    def bf16(self, n):
        m = (n + 1) // 2
        return self.f32(m).bitcast(BF16)[:, 0:n]


class Prog:
    def __init__(self, stage, stop_after=None, npass=1):
        self.npass = npass
        self.stage = stage
        self.layers = [0, 1] if stage == "A" else [2, 3]
        self.stop_after = stop_after
        nc = self.nc = bass.Bass("TRN2", target_bir_lowering=False)
        self.fw = FW(nc)
        dt = lambda name, shape, ty=F32, kind="ExternalInput": nc.dram_tensor(name, list(shape), ty, kind=kind).ap()
        self.io = [dict() for _ in range(npass)]
        for p in range(npass):
            self.io[p]["x_d"] = dt(f"x{p}", [TOK, D])
            self.io[p]["cl_d"] = dt(f"cl{p}", [128, KC])
        self.ada_w = dt("ada_w", [2, D, 6 * D])
        self.ada_b = dt("ada_b", [2, 6 * D])
        self.ada_bT = dt("ada_bT", [2, 128, 96])
        self.ln_g = dt("ln_g", [2, 2, D])
        self.ln_b = dt("ln_b", [2, 2, D])
        self.w_r = dt("w_r", [2, D, NE])
        self.b_rT = dt("b_rT", [2, NE, 1])
        self.w_gu = dt("w_gu", [2, NE, D, 1024])
        self.b_guT = dt("b_guT", [2, 128, NE, 8])
        self.w_dn = dt("w_dn", [2, NE, 512, D])
        self.b_dn = dt("b_dn", [2, NE, D])
        if stage == "A":
            self.w_in = dt("w_in", [2, D, 2 * D])
            self.b_in = dt("b_in", [2, 2 * D])
            self.b_inuT = dt("b_inuT", [2, 128, KC])
            self.lnv_gT = dt("lnv_gT", [2, 128, KC])
            self.lnv_bT = dt("lnv_bT", [2, 128, KC])
            self.w_s = dt("w_s", [2, 16, 128, 128])
            self.b_s = dt("b_s", [2, 16 * 128])
            self.w_o = dt("w_o", [2, D, D])
            self.kv_ada_w = dt("kv_ada_w", [D, 2 * D])
            self.kv_ada_bT = dt("kv_ada_bT", [128, 32])
            self.w_kv = dt("w_kv", [D, 2 * D])
            for p in range(npass):
                self.io[p]["kt_o"] = dt(f"kt_o{p}", [16, 128, TOK], BF16, "ExternalOutput")
                self.io[p]["v_o"] = dt(f"v_o{p}", [TOK, D], BF16, "ExternalOutput")
                self.io[p]["km_o"] = dt(f"km_o{p}", [128, 16, 4], F32, "ExternalOutput")
        else:
            self.w_q = dt("w_q", [2, D, D])
            self.w_ao = dt("w_ao", [2, D, D])
            for p in range(npass):
                self.io[p]["kt_all"] = dt(f"kt_all{p}", [16, 128, 4096], BF16)
                self.io[p]["v_all"] = dt(f"v_all{p}", [16, 128, 32, 128], BF16)
                self.io[p]["kt_own"] = dt(f"kt_own{p}", [16, 128, TOK], BF16)
                self.io[p]["v_own"] = dt(f"v_own{p}", [16, 128, NT, 128], BF16)
                self.io[p]["km_all"] = dt(f"km_all{p}", [128, 16, 16])
                self.io[p]["pastm"] = dt(f"pastm{p}", [128, NT, 16])
        for p in range(npass):
            self.io[p]["y_d"] = dt(f"y{p}", [TOK, D], F32, "ExternalOutput")

        sb = nc.alloc_sbuf_tensor
        self.X = sb("X", [128, NT, D], F32)
        self.WR = sb("WR", [128, NSLOT, 8192], BF16)
        self.A0 = sb("A0", [128, 16384], BF16)
        self.ident = sb("ident", [128, 128], F32)
        self.ones_bf = sb("ones_bf", [128, 128], BF16)
        self.cact = sb("cact", [128, KC], F32)
        self.cact_bf = sb("cact_bf", [128, KC], BF16)
        self.cact_rep = sb("cact_rep", [128, KC, 128], BF16)
        self.modP = sb("modP", [128, 4, KC], F32)
        self.adabT = sb("adabT", [128, 96], F32)
        self.small = sb("small", [128, 64], F32)
        ARN = 14700
        self.arena = Arena(sb("ARENA", [128, ARN], F32), ARN)
        self.ps = [nc.alloc_psum_tensor(f"ps{i}", [128, 512], F32) for i in range(8)]
        self.ring = Ring(self.fw, self.WR)
        self.out_toks = []

    def PK(self, i):
        return ("ps", i)

    def HT(self):
        return self.A0[:, :].rearrange("p (k t) -> p k t", k=KC)

    def set_pass(self, p):
        for k, v in self.io[p].items():
            setattr(self, k, v)

    def init_consts(self, first=True):
        nc, fw = self.nc, self.fw
        if first:
            fw.op("pool", lambda: nc.gpsimd.memset(self.ident[:], 1.0), W=["ident"])
            fw.op("pool", lambda: nc.gpsimd.affine_select(self.ident[:], self.ident[:], pattern=[[-1, 128]],
                                                          compare_op=ALU.is_equal, fill=0.0, base=0,
                                                          channel_multiplier=1), R=["ident"], W=["ident"])
            fw.op("pool", lambda: nc.gpsimd.memset(self.ones_bf[:], 1.0), W=["ones"])
        fw.dma("sp", self.cact[:], self.cl_d, "c", W=["cact"])
        fw.op("act", lambda: nc.scalar.activation(self.cact[:], self.cact[:], AF.Silu), R=["cact"], W=["cact"])
        fw.op("dve", lambda: nc.vector.tensor_copy(self.cact_bf[:], self.cact[:]), R=["cact"], W=["cactbf"])
        for kc in range(KC):
            fw.op("dve", lambda kc=kc: nc.vector.tensor_scalar(self.cact_rep[:, kc, :], self.ones_bf[:],
                                                               self.cact[:, kc:kc + 1], None, op0=ALU.mult),
                  R=["cact", "ones"], W=["cactrep"])
        for t in range(NT):
            fw.dma("sp", self.X[:, t, :], self.x_d[t * 128:(t + 1) * 128, :], f"x{t}", W=[("X", t)])

    def ada_piece_loader(self, w3, col0, chan_tag):
        src = w3.rearrange("(k p) f -> p k f", p=128)[:, :, col0:col0 + 512]
        def loader(slot, s):
            dst = slot.rearrange("p (k f) -> p k f", k=KC)
            self.fw.dma("pool", dst, src, f"wr{s}", W=[("WR", s)])
        return loader

    def modsP(self, wmat, vecs, bT, dst_cols):
        nc, fw = self.nc, self.fw
        for (vi, add1), dst in zip(vecs, dst_cols):
            bank = self.ps[6]
            for n in range(4):
                slot, wk = self.ring.next(("adaP", vi, n), self.ada_piece_loader(wmat, vi * D + n * 512, "a"))
                w = slot.rearrange("p (k f) -> p k f", k=KC)
                for c in range(4):
                    col = n * 4 + c
                    for kc in range(KC):
                        fw.mm1(bank[:, col:col + 1], w[:, kc, c * 128:(c + 1) * 128], self.cact_bf[:, kc:kc + 1],
                               start=(kc == 0), stop=(kc == KC - 1), R=[wk, "cactbf"], W=[self.PK(6)],
                               last=(kc == KC - 1 and c == 3))
            fw.op("dve", lambda dst=dst, vi=vi: nc.vector.tensor_tensor(dst, bank[:, 0:KC], bT[:, vi * KC:(vi + 1) * KC], op=ALU.add),
                  R=[self.PK(6), "adabT"], W=["modP"])
            if add1:
                fw.op("dve", lambda dst=dst: nc.vector.tensor_scalar(dst, dst, 1.0, None, op0=ALU.add), R=["modP"], W=["modP"])

    def modsBC(self, l, vi, dst, tmp2):
        nc, fw = self.nc, self.fw
        for n in range(4):
            slot, wk = self.ring.next(("adaB", l, vi, n), self.ada_piece_loader(self.ada_w[l], vi * D + n * 512, "a"))
            w = slot.rearrange("p (k f) -> p k f", k=KC)
            tb = tmp2[n % 2]
            fw.dma("sp", tb, self.ada_b[l, vi * D + n * 512: vi * D + (n + 1) * 512].partition_broadcast(128),
                   f"adab{n % 2}", W=[("adab", n % 2)])
            bk = 4 + (n % 2)
            fw.mm(self.ps[bk][:, :], [(self.cact_rep[:, kc, :], w[:, kc, :]) for kc in range(KC)],
                  R=[wk, "cactrep"], W=[self.PK(bk)])
            fw.op("dve", lambda n=n, tb=tb, bk=bk: nc.vector.scalar_tensor_tensor(dst[:, n * 512:(n + 1) * 512], self.ps[bk][:, :], 1.0, tb,
                                                                                op0=ALU.add, op1=ALU.add),
                  R=[self.PK(bk), ("adab", n % 2)], W=[("gbc", n)])

    def build_ht(self, tiles, sc, bi, dst, ntg, router=None):
        nc, fw = self.nc, self.fw
        k = 0
        for kc in range(KC):
            for tg in range(ntg):
                bk = 6 + (k % 2)
                k += 1
                bank = self.ps[bk]
                for i in range(4):
                    t = tiles[tg * 4 + i]
                    fw.tr(bank[:, i * 128:(i + 1) * 128], self.X[:, t, kc * 128:(kc + 1) * 128], self.ident[:],
                          R=[("X", t), "ident"], W=[self.PK(bk)], last=(i == 3))
                fw.op("act", lambda kc=kc, tg=tg, bank=bank: nc.scalar.activation(
                    dst[:, kc, tg * 512:(tg + 1) * 512], bank[:, :], AF.Identity,
                    bias=bi[:, kc:kc + 1], scale=sc[:, kc:kc + 1]),
                    R=[self.PK(bk), "modP"], W=[("HT", kc, tg)])
                if router is not None:
                    hf, wr32, lt = router
                    hb = hf[(kc * ntg + tg) % 2]
                    hk = ("hf", (kc * ntg + tg) % 2)
                    fw.op("dve", lambda kc=kc, bank=bank, hb=hb: nc.vector.tensor_scalar(
                        hb, bank[:, :], sc[:, kc:kc + 1], bi[:, kc:kc + 1], op0=ALU.mult, op1=ALU.add),
                        R=[self.PK(bk), "modP"], W=[hk])
                    fw.mm(self.ps[4 + tg][:, :], [(wr32[:, kc, :], hb)], R=[hk, "wr32"], W=[self.PK(4 + tg)])
                    lts = lt[0:NE, tg * 512:(tg + 1) * 512]
                    if kc == 0:
                        fw.op("dve", lambda tg=tg, lts=lts: nc.vector.tensor_copy(lts, self.ps[4 + tg][0:NE, :]), R=[self.PK(4 + tg)], W=[("LTs", tg)])
                    else:
                        fw.op("dve", lambda tg=tg, lts=lts: nc.vector.tensor_tensor(lts, lts, self.ps[4 + tg][0:NE, :], op=ALU.add),
                              R=[self.PK(4 + tg), ("LTs", tg)], W=[("LTs", tg)])

    def layer_norm(self, tiles, g_ap, b_ap, gbc, bbc, st, mv):
        nc, fw = self.nc, self.fw
        fw.dma("sp", gbc, g_ap.partition_broadcast(128), "lng", W=["lng"])
        fw.dma("sp", bbc, b_ap.partition_broadcast(128), "lnb", W=["lnb"])
        for t in tiles:
            xt = self.X[:, t, :]
            xk = ("X", t)
            for c in range(4):
                fw.op("dve", lambda c=c, xt=xt: nc.vector.bn_stats(st[:, c, :], xt[:, c * 512:(c + 1) * 512]), R=[xk], W=[("st", c)])
            fw.op("dve", lambda: nc.vector.bn_aggr(mv[:, 0:2], st[:, :, :]), R=[("st", c) for c in range(4)], W=["mv"])
            fw.op("dve", lambda: nc.vector.tensor_scalar(mv[:, 2:3], mv[:, 1:2], EPS, None, op0=ALU.add), R=["mv"], W=["mv2"])
            fw.op("act", lambda: nc.scalar.activation(mv[:, 2:3], mv[:, 2:3], AF.Sqrt), R=["mv2"], W=["mv2"])
            fw.op("dve", lambda: nc.vector.reciprocal(mv[:, 2:3], mv[:, 2:3]), R=["mv2"], W=["mv2"])
            fw.op("dve", lambda: nc.vector.scalar_tensor_tensor(mv[:, 3:4], mv[:, 0:1], -1.0, mv[:, 2:3], op0=ALU.mult, op1=ALU.mult),
                  R=["mv", "mv2"], W=["mv3"])
            fw.op("act", lambda xt=xt: nc.scalar.activation(xt, xt, AF.Identity, bias=mv[:, 3:4], scale=mv[:, 2:3]),
                  R=[xk, "mv2", "mv3"], W=[xk])
            fw.op("dve", lambda xt=xt: nc.vector.tensor_tensor(xt, xt, gbc, op=ALU.mult), R=[xk, "lng"], W=[xk])
            fw.op("dve", lambda xt=xt: nc.vector.tensor_tensor(xt, xt, bbc, op=ALU.add), R=[xk, "lnb"], W=[xk])

    def resid_evac(self, bk, t, n, gbc, tmp, tk):
        nc, fw = self.nc, self.fw
        xs = self.X[:, t, n * 512:(n + 1) * 512]
        fw.op("dve", lambda: nc.vector.tensor_tensor(tmp, self.ps[bk][:, :], gbc[:, n * 512:(n + 1) * 512], op=ALU.mult),
              R=[self.PK(bk), ("gbc", n)], W=[tk])
        fw.op("dve", lambda: nc.vector.scalar_tensor_tensor(xs, xs, ALPHA, tmp, op0=ALU.mult, op1=ALU.add),
              R=[("X", t), tk], W=[("X", t)])

    def gmlp(self, li):
        nc, fw, ar = self.nc, self.fw, self.arena
        fw.barrier()
        ar.off = 0
        st = ar.f32(24).rearrange("p (c s) -> p c s", c=4)
        mv = ar.f32(4)
        mark = ar.off
        VB = ar.bf16(4 * D).rearrange("p (t f) -> p t f", t=4)
        g1bc = ar.f32(D)
        Bt = ar.f32(D).rearrange("p (g t) -> p g t", g=16)
        WST = ar.bf16(D).rearrange("p (g t) -> p g t", g=16)
        binv = ar.bf16(D)
        adab = [ar.f32(512), ar.f32(512)]
        tmp = [ar.f32(512), ar.f32(512)]
        binu = ar.f32(KC)
        lnvg = ar.f32(KC)
        lnvb = ar.f32(KC)
        a0f = self.A0[:, :].bitcast(F32)
        wss = a0f[:, 0:2048].rearrange("p (g s) -> p g s", g=16)
        bsbc = a0f[:, 2048:4096].rearrange("p (g t) -> p g t", g=16)
        fw.dma("sp", wss, self.w_s[li].rearrange("g t s -> t g s"), "wss", W=["wss"])
        fw.dma("sp", bsbc, self.b_s[li].partition_broadcast(128), "bsbc", W=["bsbc"])
        fw.dma("pool", binv[0:1, :], self.b_in[li:li + 1, D:2 * D], "binv", W=["binv"])
        fw.dma("sp", binu, self.b_inuT[li], "binu", W=["binu"])
        fw.dma("sp", lnvg, self.lnv_gT[li], "lnvg", W=["lnvg"])
        fw.dma("sp", lnvb, self.lnv_bT[li], "lnvb", W=["lnvb"])
        fw.dma("sp", self.adabT[:], self.ada_bT[li], "adabT", W=["adabT"])
        fw.op("pool", lambda: nc.gpsimd.affine_select(wss, wss, pattern=[[0, 16], [-1, 128]], compare_op=ALU.is_ge,
                                                      fill=0.0, base=0, channel_multiplier=1), R=["wss"], W=["wss"])
        for gq in range(4):
            bk = 6 + gq % 2
            for i in range(4):
                g = gq * 4 + i
                fw.tr(self.ps[bk][:, i * 128:(i + 1) * 128], wss[:, g, :], self.ident[:], R=["wss", "ident"], W=[self.PK(bk)], last=(i == 3))
            fw.op("act", lambda gq=gq, bk=bk: nc.scalar.copy(WST[:, gq * 4:(gq + 1) * 4, :], self.ps[bk][:, :].rearrange("p (g t) -> p g t", g=4)),
                  R=[self.PK(bk)], W=[("WST", gq)])
        for gq in range(4):
            bk = 6 + gq % 2
            fw.mm(self.ps[bk][:, :], [(self.ones_bf[:], WST[:, gq * 4:(gq + 1) * 4, :])], R=[("WST", gq), "ones"], W=[self.PK(bk)])
            for i in range(4):
                g = gq * 4 + i
                fw.op("dve", lambda g=g, i=i, bk=bk: nc.vector.scalar_tensor_tensor(
                    Bt[:, g, :], self.ps[bk][:, i * 128:(i + 1) * 128], lnvb[:, g:g + 1], bsbc[:, g, :], op0=ALU.mult, op1=ALU.add),
                    R=[self.PK(bk), "lnvb", "bsbc"], W=[("Bt", g)])
        self.modsP(self.ada_w[li], [(0, False), (1, True)], self.adabT, [self.modP[:, 0, :], self.modP[:, 1, :]])
        self.modsBC(li, 2, g1bc, adab)
        fw.barrier()
        HTh = self.A0[:, 0:8192].rearrange("p (k t) -> p k t", k=KC)
        UT = self.A0[:, 8192:16384].rearrange("p (k t) -> p k t", k=KC)
        w_in3 = self.w_in[li].rearrange("(k p) f -> p k f", p=128)
        w_o3 = self.w_o[li].rearrange("(k p) f -> p k f", p=128)
        for half in range(2):
            tiles = [half * 4 + i for i in range(4)]
            self.build_ht(tiles, self.modP[:, 1, :], self.modP[:, 0, :], HTh, 1)
            htk = [("HT", kc, 0) for kc in range(KC)]
            k = 0
            for n in range(4):
                def ld(slot, s, n=n):
                    fw.dma("pool", slot.rearrange("p (k f) -> p k f", k=KC), w_in3[:, :, D + n * 512: D + (n + 1) * 512], f"wr{s}", W=[("WR", s)])
                slot, wk = self.ring.next(("wv", li, half, n), ld)
                w = slot.rearrange("p (k f) -> p k f", k=KC)
                for i in range(4):
                    bk = k % 4
                    k += 1
                    pairs = [(HTh[:, kc, i * 128:(i + 1) * 128], w[:, kc, :]) for kc in range(KC)]
                    pairs.append((self.ones_bf[0:1, :], binv[0:1, n * 512:(n + 1) * 512]))
                    fw.mm(self.ps[bk][:, :], pairs, R=[wk, "ones", "binv"] + htk, W=[self.PK(bk)])
                    fw.op("act", lambda i=i, n=n, bk=bk: nc.scalar.activation(VB[:, i, n * 512:(n + 1) * 512], self.ps[bk][:, :], AF.Gelu_apprx_tanh),
                          R=[self.PK(bk)], W=[("VB", i, n)])
            for i in range(4):
                for n in range(4):
                    fw.op("dve", lambda i=i, n=n: nc.vector.bn_stats(st[:, n, :], VB[:, i, n * 512:(n + 1) * 512]), R=[("VB", i, n)], W=[("st", n)])
                fw.op("dve", lambda: nc.vector.bn_aggr(mv[:, 0:2], st[:, :, :]), R=[("st", c) for c in range(4)], W=["mv"])
                fw.op("dve", lambda: nc.vector.tensor_scalar(mv[:, 2:3], mv[:, 1:2], EPS, None, op0=ALU.add), R=["mv"], W=["mv2"])
                fw.op("act", lambda: nc.scalar.activation(mv[:, 2:3], mv[:, 2:3], AF.Sqrt), R=["mv2"], W=["mv2"])
                fw.op("dve", lambda: nc.vector.reciprocal(mv[:, 2:3], mv[:, 2:3]), R=["mv2"], W=["mv2"])
                fw.op("dve", lambda: nc.vector.scalar_tensor_tensor(mv[:, 3:4], mv[:, 0:1], -1.0, mv[:, 2:3], op0=ALU.mult, op1=ALU.mult),
                      R=["mv", "mv2"], W=["mv3"])
                fw.op("act", lambda i=i: nc.scalar.activation(VB[:, i, :], VB[:, i, :], AF.Identity, bias=mv[:, 3:4], scale=mv[:, 2:3]),
                      R=[("VB", i, n) for n in range(4)] + ["mv2", "mv3"], W=[("VB", i, n) for n in range(4)])
            for q in range(4):
                def ld(slot, s, q=q):
                    dst = slot.rearrange("p (c k f) -> p c k f", c=4, k=KC)
                    for c in range(4):
                        fw.dma("pool", dst[:, c], w_in3[:, :, (q * 4 + c) * 128:(q * 4 + c + 1) * 128], f"wr{s}", W=[("WR", s)])
                slot, wk = self.ring.next(("wu", li, half, q), ld)
                w = slot.rearrange("p (c k f) -> p c k f", c=4, k=KC)
                for c in range(4):
                    ch = q * 4 + c
                    bk = k % 4
                    k += 1
                    fw.mm(self.ps[bk][:, :], [(w[:, c, kc, :], HTh[:, kc, :]) for kc in range(KC)], R=[wk] + htk, W=[self.PK(bk)])
                    fw.op("act", lambda ch=ch, bk=bk: nc.scalar.activation(UT[:, ch, :], self.ps[bk][:, :], AF.Gelu_apprx_tanh, bias=binu[:, ch:ch + 1]),
                          R=[self.PK(bk), "binu"], W=[("UT", ch)])
            for i in range(4):
                for gq in range(4):
                    bk = k % 4
                    k += 1
                    for j in range(4):
                        g = gq * 4 + j
                        fw.mm1(self.ps[bk][:, j * 128:(j + 1) * 128], VB[:, i, g * 128:(g + 1) * 128], WST[:, g, :], start=True, stop=True,
                               R=[("VB", i, g // 4), ("WST", gq)], W=[self.PK(bk)], last=(j == 3))
                    tb = tmp[k % 2]
                    tk = ("tmp", k % 2)
                    for j in range(4):
                        g = gq * 4 + j
                        fw.op("dve", lambda g=g, j=j, bk=bk, tb=tb: nc.vector.scalar_tensor_tensor(
                            tb[:, j * 128:(j + 1) * 128], self.ps[bk][:, j * 128:(j + 1) * 128], lnvg[:, g:g + 1], Bt[:, g, :], op0=ALU.mult, op1=ALU.add),
                            R=[self.PK(bk), "lnvg", ("Bt", g)], W=[tk])
                    uview = UT[:, gq * 4:(gq + 1) * 4, i * 128:(i + 1) * 128]
                    fw.op("dve", lambda uview=uview, tb=tb: nc.vector.tensor_tensor(uview, tb.rearrange("p (g t) -> p g t", g=4), uview, op=ALU.mult),
                          R=[tk] + [("UT", gq * 4 + j) for j in range(4)], W=[("UT", gq * 4 + j) for j in range(4)])
            utk = [("UT", c) for c in range(KC)]
            for n in range(4):
                def ld(slot, s, n=n):
                    fw.dma("pool", slot.rearrange("p (k f) -> p k f", k=KC), w_o3[:, :, n * 512:(n + 1) * 512], f"wr{s}", W=[("WR", s)])
                slot, wk = self.ring.next(("wo", li, half, n), ld)
                w = slot.rearrange("p (k f) -> p k f", k=KC)
                for i in range(4):
                    bk = k % 4
                    k += 1
                    fw.mm(self.ps[bk][:, :], [(UT[:, kc, i * 128:(i + 1) * 128], w[:, kc, :]) for kc in range(KC)], R=[wk] + utk, W=[self.PK(bk)])
                    self.resid_evac(bk, tiles[i], n, g1bc, tmp[k % 2], ("tmp", k % 2))
        fw.barrier()
        ar.off = mark
        gbc = ar.f32(D)
        bbc = ar.f32(D)
        self.layer_norm(range(NT), self.ln_g[li, 0], self.ln_b[li, 0], gbc, bbc, st, mv)

    def moe(self, li):
        nc, fw, ar = self.nc, self.fw, self.arena
        fw.barrier()
        ar.off = 0
        g2bc = ar.f32(D)
        DT = [ar.f32(512), ar.f32(512)]
        G = ar.f32(NT * NE).rearrange("p (t e) -> p t e", t=NT)
        Gp = ar.f32(NT * NE).rearrange("p (t e) -> p t e", t=NT)
        bgu = ar.f32(NE * 8).rearrange("p (e c) -> p e c", e=NE)
        st = ar.f32(24).rearrange("p (c s) -> p c s", c=4)
        mv = ar.f32(4)
        mark = ar.off
        ACTT = [ar.bf16(4096).rearrange("p (j t) -> p j t", j=4) for _ in range(2)]
        TT = [ar.f32(512), ar.f32(512)]
        UU = [ar.f32(512), ar.f32(512)]
        g2bf = ar.bf16(D)
        ar.off = mark
        hf = [ar.f32(512), ar.f32(512)]
        wr32 = ar.f32(KC * 128).rearrange("p (k e) -> p k e", k=KC)
        bd32 = ar.f32(D)
        LTs = ar.f32(TOK)
        L = ar.f32(NT * NE).rearrange("p (t e) -> p t e", t=NT)
        MS = ar.f32(NT * NE).rearrange("p (t e) -> p t e", t=NT)
        m8 = ar.f32(8)
        sm = ar.f32(4)
        GT = ar.f32(TOK).rearrange("p (t k) -> p t k", t=NT)
        adab = [ar.f32(512), ar.f32(512)]
        brt = ar.f32(1)
        fw.dma("sp", self.adabT[:], self.ada_bT[li], "adabT", W=["adabT"])
        fw.op("pool", lambda: nc.gpsimd.memset(wr32, 0.0), W=["wr32"])
        fw.dma("sp", wr32[:, :, 0:NE], self.w_r[li].rearrange("(k p) e -> p k e", p=128), "wr32", R=["wr32"], W=["wr32"])
        fw.dma("sp", bd32[0:NE, :], self.b_dn[li], "bd32", W=["bd32"])
        fw.dma("sp", brt[0:NE, :], self.b_rT[li], "brt", W=["brt"])
        fw.dma("sp", bgu, self.b_guT[li], "bgu", W=["bgu"])
        import os
        PRO = int(os.environ.get("MOE_PRO", 99))
        if PRO <= 0:
            return
        fw.op("dve", lambda: nc.vector.tensor_scalar(bgu[:, :, 0:4], bgu[:, :, 0:4], SW_A, None, op0=ALU.mult), R=["bgu"], W=["bgu"])
        fw.op("dve", lambda: nc.vector.tensor_scalar(bgu[:, :, 4:8], bgu[:, :, 4:8], 1.0, None, op0=ALU.add), R=["bgu"], W=["bgu"])
        self.modsP(self.ada_w[li], [(3, False), (4, True)], self.adabT, [self.modP[:, 2, :], self.modP[:, 3, :]])
        if PRO <= 1:
            return
        self.modsBC(li, 5, g2bc, adab)
        HT = self.HT()
        if PRO <= 2:
            return
        self.build_ht(list(range(NT)), self.modP[:, 3, :], self.modP[:, 2, :], HT, 2, router=(hf, wr32, LTs))
        if PRO <= 3:
            return
        for tg in range(2):
            fw.op("act", lambda tg=tg: nc.scalar.activation(LTs[0:NE, tg * 512:(tg + 1) * 512], LTs[0:NE, tg * 512:(tg + 1) * 512], AF.Identity, bias=brt[0:NE, 0:1]),
                  R=[("LTs", tg), "brt"], W=[("LTs", tg)])
        for t in range(NT):
            fw.tr(self.ps[6][:, t * NE:(t + 1) * NE], LTs[0:NE, t * 128:(t + 1) * 128], self.ident[0:NE, 0:NE],
                  R=[("LTs", t // 4), "ident"], W=[self.PK(6)], last=(t == NT - 1))
        fw.op("dve", lambda: nc.vector.tensor_copy(L, self.ps[6][:, 0:NT * NE].rearrange("p (t e) -> p t e", t=NT)), R=[self.PK(6)], W=["L"])
        if PRO <= 4:
            return
        for t in range(NT):
            Lt, Mt, Gt, Gpt = L[:, t, :], MS[:, t, :], G[:, t, :], Gp[:, t, :]
            fw.op("dve", lambda Lt=Lt: nc.vector.max(m8, Lt), R=["L"], W=["m8"])
            fw.op("dve", lambda Lt=Lt, Mt=Mt: nc.vector.tensor_scalar(Mt, Lt, m8[:, 3:4], None, op0=ALU.is_ge), R=["L", "m8"], W=["MS"])
            fw.op("dve", lambda: nc.vector.tensor_scalar(sm[:, 0:1], m8[:, 0:1], -1.0, None, op0=ALU.mult), R=["m8"], W=["sm0"])
            fw.op("act", lambda Lt=Lt, Gt=Gt: nc.scalar.activation(Gt, Lt, AF.Exp, bias=sm[:, 0:1], scale=1.0), R=["L", "sm0"], W=["G"])
            fw.op("dve", lambda Gt=Gt, Mt=Mt: nc.vector.tensor_tensor(Gt, Gt, Mt, op=ALU.mult), R=["G", "MS"], W=["G"])
            fw.op("dve", lambda Gt=Gt: nc.vector.reduce_sum(sm[:, 1:2], Gt, axis=AX.X), R=["G"], W=["sm1"])
            fw.op("dve", lambda: nc.vector.reciprocal(sm[:, 2:3], sm[:, 1:2]), R=["sm1"], W=["sm2"])
            fw.op("dve", lambda Gt=Gt: nc.vector.tensor_scalar(Gt, Gt, sm[:, 2:3], None, op0=ALU.mult), R=["G", "sm2"], W=["G"])
            fw.op("dve", lambda Gt=Gt, Gpt=Gpt: nc.vector.tensor_scalar(Gpt, Gt, 1.0 / SW_A, None, op0=ALU.mult), R=["G"], W=["Gp"])
        if PRO <= 5:
            return
        for t in range(NT):
            bk = 6 + (t // 4)
            fw.tr(self.ps[bk][0:NE, (t % 4) * 128:(t % 4 + 1) * 128], G[:, t, :], self.ident[:], R=["G", "ident"], W=[self.PK(bk)], last=(t % 4 == 3))
        for h in range(2):
            fw.op("act", lambda h=h: nc.scalar.copy(GT[0:NE, h * 4:(h + 1) * 4, :], self.ps[6 + h][0:NE, :].rearrange("p (t k) -> p t k", t=4)),
                  R=[self.PK(6 + h)], W=[("GT", h)])
        if PRO <= 6:
            return
        k = 0
        for t in range(NT):
            for n in range(4):
                bk = 4 + k % 2
                fw.mm(self.ps[bk][:, :], [(GT[0:NE, t, :], bd32[0:NE, n * 512:(n + 1) * 512])], R=[("GT", t // 4), "bd32"], W=[self.PK(bk)])
                self.resid_evac(bk, t, n, g2bc, DT[k % 2], ("DT", k % 2))
                k += 1
        fw.barrier()
        fw.op("dve", lambda: nc.vector.tensor_copy(g2bf, g2bc), R=[("gbc", n) for n in range(4)], W=["g2bf"])
        w_gu = self.w_gu[li]
        w_dn = self.w_dn[li]
        htk = [[("HT", kc, th) for kc in range(KC)] for th in range(2)]

        def gu_unit(e, j, state):
            def ld(slot, s, e=e, j=j):
                dst = slot[:, 0:4096].rearrange("p (h k f) -> p h k f", h=2, k=KC)
                src = w_gu[e].rearrange("(k p) f -> p k f", p=128)
                for h in range(2):
                    c0 = h * 512 + j * 128
                    fw.dma("pool", dst[:, h], src[:, :, c0:c0 + 128], f"wr{s}", W=[("WR", s)])
            slot, wk = self.ring.next(("gu", li, e, j), ld)
            w4 = slot[:, 0:4096].rearrange("p (h k f) -> p h k f", h=2, k=KC)
            eb = e % 2
            for th in range(2):
                fw.mm(self.ps[th * 2][:, :], [(w4[:, 0, kc, :], HT[:, kc, th * 512:(th + 1) * 512]) for kc in range(KC)],
                      R=[wk] + htk[th], W=[self.PK(th * 2)])
                fw.mm(self.ps[th * 2 + 1][:, :], [(w4[:, 1, kc, :], HT[:, kc, th * 512:(th + 1) * 512]) for kc in range(KC)],
                      R=[wk] + htk[th], W=[self.PK(th * 2 + 1)])
                fw.op("act", lambda th=th: nc.scalar.activation(TT[th], self.ps[th * 2][:, :], AF.Silu, bias=bgu[:, e, j:j + 1], scale=SW_A),
                      R=[self.PK(th * 2), "bgu"], W=[("TT", th)])
                fw.op("act", lambda th=th: nc.scalar.activation(UU[th], self.ps[th * 2 + 1][:, :], AF.Identity, bias=bgu[:, e, 4 + j:5 + j]),
                      R=[self.PK(th * 2 + 1), "bgu"], W=[("UU", th)])
                fw.op("dve", lambda th=th: nc.vector.tensor_scalar(UU[th], UU[th], 8.0, -6.0, op0=ALU.min, op1=ALU.max), R=[("UU", th)], W=[("UU", th)])
                fw.op("dve", lambda th=th: nc.vector.scalar_tensor_tensor(ACTT[eb][:, j, th * 512:(th + 1) * 512], TT[th], C7, UU[th], op0=ALU.min, op1=ALU.mult),
                      R=[("TT", th), ("UU", th)], W=[("ACTT", eb, j)])

        def down_unit(e, state):
            def ld(slot, s, e=e):
                fw.dma("pool", slot.rearrange("p (j n) -> p j n", j=4), w_dn[e].rearrange("(j p) n -> p j n", p=128), f"wr{s}", W=[("WR", s)])
            slot, wk = self.ring.next(("dn", li, e), ld)
            w = slot.rearrange("p (j n) -> p j n", j=4)
            eb = e % 2
            k = state.get("dk", 0)
            for j in range(4):
                fw.op("dve", lambda j=j: nc.vector.tensor_tensor(w[:, j, :], w[:, j, :], g2bf, op=ALU.mult), R=[wk, "g2bf"], W=[wk])
            for t in range(NT):
                for n in range(4):
                    bk = 4 + k % 4
                    k += 1
                    fw.mm(self.ps[bk][:, :], [(ACTT[eb][:, j, t * 128:(t + 1) * 128], w[:, j, n * 512:(n + 1) * 512]) for j in range(4)],
                          R=[wk] + [("ACTT", eb, j) for j in range(4)], W=[self.PK(bk)])
                    xs = self.X[:, t, n * 512:(n + 1) * 512]
                    fw.op("dve", lambda bk=bk, xs=xs, t=t: nc.vector.scalar_tensor_tensor(
                        xs, self.ps[bk][:, :], Gp[:, t, e:e + 1], xs, op0=ALU.mult, op1=ALU.add),
                        R=[self.PK(bk), "Gp", ("X", t)], W=[("X", t)])
            state["dk"] = k

        state = {}
        import os
        nexp = int(os.environ.get("MOE_NEXP", NE))
        for e in range(nexp):
            gu_unit(e, 0, state)
            if e > 0:
                down_unit(e - 1, state)
            for j in range(1, 4):
                gu_unit(e, j, state)
        if nexp > 0:
            down_unit(nexp - 1, state)
        fw.barrier()
        ar.off = mark
        gbc = ar.f32(D)
        bbc = ar.f32(D)
        self.layer_norm(range(NT), self.ln_g[li, 1], self.ln_b[li, 1], gbc, bbc, st, mv)

    def kv(self):
        nc, fw, ar = self.nc, self.fw, self.arena
        fw.barrier()
        ar.off = 0
        KTs = [ar.bf16(TOK), ar.bf16(TOK)]
        Vs = [ar.bf16(512), ar.bf16(512)]
        kms = ar.f32(64).rearrange("p (h b) -> p h b", h=16)
        kvb = ar.f32(32)
        modKV = ar.f32(32).rearrange("p (v k) -> p v k", v=2)
        fw.dma("sp", kvb, self.kv_ada_bT, "kvb", W=["adabT"])
        self.modsP(self.kv_ada_w, [(0, False), (1, True)], kvb, [modKV[:, 0, :], modKV[:, 1, :]])
        HT = self.HT()
        self.build_ht(list(range(NT)), modKV[:, 1, :], modKV[:, 0, :], HT, 2)
        htk = [[("HT", kc, th) for kc in range(KC)] for th in range(2)]
        w3 = self.w_kv.rearrange("(k p) f -> p k f", p=128)
        k = 0
        for q in range(4):
            def ld(slot, s, q=q):
                dst = slot.rearrange("p (c k f) -> p c k f", c=4, k=KC)
                for c in range(4):
                    fw.dma("pool", dst[:, c], w3[:, :, (q * 4 + c) * 128:(q * 4 + c + 1) * 128], f"wr{s}", W=[("WR", s)])
            slot, wk = self.ring.next(("wk", q), ld)
            w = slot.rearrange("p (c k f) -> p c k f", c=4, k=KC)
            for c in range(4):
                h = q * 4 + c
                kb = KTs[h % 2]
                kk = ("KTs", h % 2)
                for th in range(2):
                    bk = k % 4
                    k += 1
                    fw.mm(self.ps[bk][:, :], [(w[:, c, kc, :], HT[:, kc, th * 512:(th + 1) * 512]) for kc in range(KC)], R=[wk] + htk[th], W=[self.PK(bk)])
                    fw.op("act", lambda kb=kb, th=th, bk=bk: nc.scalar.copy(kb[:, th * 512:(th + 1) * 512], self.ps[bk][:, :]), R=[self.PK(bk)], W=[kk])
                    fw.op("dve", lambda h=h, th=th, bk=bk: nc.vector.tensor_reduce(kms[:, h, th * 2:(th + 1) * 2], self.ps[bk][:, :].rearrange("p (b s) -> p b s", b=2),
                                                                                   op=ALU.add, axis=AX.X), R=[self.PK(bk)], W=["kms"])
                self.out_toks.append(fw.dma("sp", self.kt_o[h], kb, f"kto{h % 2}", R=[kk]))
        fw.op("dve", lambda: nc.vector.tensor_scalar(kms, kms, 1.0 / 256.0, None, op0=ALU.mult), R=["kms"], W=["kms"])
        self.out_toks.append(fw.dma("sp", self.km_o, kms, "kmo", R=["kms"]))
        k2 = 0
        for n in range(4):
            def ld(slot, s, n=n):
                fw.dma("pool", slot.rearrange("p (k f) -> p k f", k=KC), w3[:, :, D + n * 512: D + (n + 1) * 512], f"wr{s}", W=[("WR", s)])
            slot, wk = self.ring.next(("wvv", n), ld)
            w = slot.rearrange("p (k f) -> p k f", k=KC)
            for t in range(NT):
                bk = k % 4
                k += 1
                vb = Vs[k2 % 2]
                vk = ("Vs", k2 % 2)
                k2 += 1
                fw.mm(self.ps[bk][:, :], [(HT[:, kc, t * 128:(t + 1) * 128], w[:, kc, :]) for kc in range(KC)], R=[wk] + htk[t // 4], W=[self.PK(bk)])
                fw.op("act", lambda vb=vb, bk=bk: nc.scalar.copy(vb, self.ps[bk][:, :]), R=[self.PK(bk)], W=[vk])
                self.out_toks.append(fw.dma("sp", self.v_o[t * 128:(t + 1) * 128, n * 512:(n + 1) * 512], vb, f"vo{k2 % 2}", R=[vk]))

    def attn(self, li):
        nc, fw, ar = self.nc, self.fw, self.arena
        fw.barrier()
        ar.off = 0
        st = ar.f32(24).rearrange("p (c s) -> p c s", c=4)
        mv = ar.f32(4)
        mark = ar.off
        QT = ar.bf16(KC * TOK).rearrange("p (h t) -> p h t", h=16)
        KMb = ar.bf16(256).rearrange("p (h n) -> p h n", h=16)
        PM = ar.f32(128)
        KTo = [ar.bf16(TOK) for _ in range(2)]
        Vo = [ar.bf16(TOK).rearrange("p (t d) -> p t d", t=NT) for _ in range(2)]
        MBTf = [ar.f32(TOK // 2) for _ in range(2)]
        MBT = [m.bitcast(BF16) for m in MBTf]
        GS = ar.f32(128)
        SEL = ar.f32(128)
        m8 = ar.f32(64).rearrange("p (t e) -> p t e", t=NT)
        thr = ar.f32(8)
        PT = [ar.bf16(256) for _ in range(3)]
        RI = [ar.f32(256) for _ in range(2)]
        ENf = ar.f32(16 * 64)
        EN = ENf.bitcast(BF16).rearrange("p (n k) -> p n k", n=16)
        TRI2 = ar.bf16(256)
        TRI3 = ar.bf16(256)
        identb = ar.bf16(128)
        fw.dma("sp", self.adabT[:], self.ada_bT[li], "adabT", W=["adabT"])
        fw.dma("pool", KMb, self.km_all, "kmb", W=["KMb"])
        fw.op("dve", lambda: nc.vector.memset(ENf, 0.0), W=["EN"])
        for hb_ in range(2):
            fw.op("dve", lambda hb_=hb_: nc.vector.memset(MBTf[hb_], 0.0), W=[("MBT", hb_, 0), ("MBT", hb_, 1)])
        fw.op("pool", lambda: nc.gpsimd.memset(EN[0:16], 1.0), R=["EN"], W=["EN"])
        fw.op("pool", lambda: nc.gpsimd.affine_select(EN[0:16], EN[0:16], pattern=[[-1, 16], [0, 128]], compare_op=ALU.is_equal,
                                                      fill=0.0, base=0, channel_multiplier=1), R=["EN"], W=["EN"])
        fw.op("pool", lambda: nc.gpsimd.memset(TRI2, 0.0), W=["TRI2"])
        fw.op("pool", lambda: nc.gpsimd.affine_select(TRI2[:, 0:128], TRI2[:, 0:128], pattern=[[1, 128]], compare_op=ALU.is_ge,
                                                      fill=NEG, base=0, channel_multiplier=-1), R=["TRI2"], W=["TRI2"])
        fw.op("pool", lambda: nc.gpsimd.memset(TRI3, NEG), W=["TRI3"])
        fw.op("pool", lambda: nc.gpsimd.tensor_copy(TRI3[:, 128:256], TRI2[:, 0:128]), R=["TRI2", "TRI3"], W=["TRI3"])
        fw.op("dve", lambda: nc.vector.tensor_copy(identb, self.ident[:]), R=["ident"], W=["identb"])
        self.modsP(self.ada_w[li], [(0, False), (1, True)], self.adabT, [self.modP[:, 0, :], self.modP[:, 1, :]])
        HT = self.HT()
        self.build_ht(list(range(NT)), self.modP[:, 1, :], self.modP[:, 0, :], HT, 2)
        htk = [[("HT", kc, th) for kc in range(KC)] for th in range(2)]
        wq3 = self.w_q[li].rearrange("(k p) f -> p k f", p=128)
        k = 0
        for q in range(4):
            def ld(slot, s, q=q):
                dst = slot.rearrange("p (c k f) -> p c k f", c=4, k=KC)
                for c in range(4):
                    fw.dma("pool", dst[:, c], wq3[:, :, (q * 4 + c) * 128:(q * 4 + c + 1) * 128], f"wr{s}", W=[("WR", s)])
            slot, wk = self.ring.next(("wq", li, q), ld)
            w = slot.rearrange("p (c k f) -> p c k f", c=4, k=KC)
            for c in range(4):
                h = q * 4 + c
                for th in range(2):
                    bk = k % 4
                    k += 1
                    fw.mm(self.ps[bk][:, :], [(w[:, c, kc, :], HT[:, kc, th * 512:(th + 1) * 512]) for kc in range(KC)], R=[wk] + htk[th], W=[self.PK(bk)])
                    fw.op("act", lambda h=h, th=th, bk=bk: nc.scalar.activation(QT[:, h, th * 512:(th + 1) * 512], self.ps[bk][:, :], AF.Identity, scale=128.0 ** -0.5),
                          R=[self.PK(bk)], W=[("QT", h)])
        fw.barrier()
        OT = self.HT()
        fw.dma("sp", PM, self.pastm.rearrange("p t n -> p (t n)"), "pm", W=["PM"])
        sk = 0
        for h in range(16):
            hb = h % 2
            def ld(slot, s, h=h, kta=self.kt_all, va=self.v_all):
                fw.dma("pool", slot[:, 0:4096], kta[h], f"wr{s}", W=[("WR", s)])
                fw.dma("pool", slot[:, 4096:8192].rearrange("p (t d) -> p t d", t=32), va[h], f"wr{s}", W=[("WR", s)])
            slot, wk = self.ring.next(("kvh", li, h), ld)
            KTa = slot[:, 0:4096]
            Va = slot[:, 4096:8192].rearrange("p (t d) -> p t d", t=32)
            fw.dma("sp", KTo[hb], self.kt_own[h], f"kto{hb}", W=[("KTo", hb)])
            fw.dma("sp", Vo[hb], self.v_own[h], f"vo{hb}", W=[("Vo", hb)])
            for t in range(NT):
                fw.mm1(self.ps[6][:, t * 16:(t + 1) * 16], QT[:, h, t * 128:(t + 1) * 128], KMb[:, h, :], start=True, stop=True,
                       R=[("QT", h), "KMb"], W=[self.PK(6)], last=(t == NT - 1))
            fw.op("dve", lambda: nc.vector.tensor_tensor(GS, self.ps[6][:, 0:128], PM, op=ALU.add), R=[self.PK(6), "PM"], W=["GS"])
            for t in range(NT):
                fw.op("dve", lambda t=t: nc.vector.max(m8[:, t, :], GS[:, t * 16:(t + 1) * 16]), R=["GS"], W=["m8"])
            fw.op("dve", lambda: nc.vector.tensor_scalar(thr, m8[:, :, 2], -1e29, None, op0=ALU.max), R=["m8"], W=["thr"])
            for t in range(NT):
                fw.op("dve", lambda t=t: nc.vector.tensor_scalar(SEL[:, t * 16:(t + 1) * 16], GS[:, t * 16:(t + 1) * 16], thr[:, t:t + 1], None, op0=ALU.is_ge),
                      R=["GS", "thr"], W=["SEL"])
            fw.op("dve", lambda: nc.vector.tensor_scalar(SEL, SEL, -1.0, -NEG, op0=ALU.add, op1=ALU.mult), R=["SEL"], W=["SEL"])
            for t in range(NT):
                bk = 7
                fw.tr(self.ps[bk][0:16, (t % 4) * 128:(t % 4 + 1) * 128], SEL[:, t * 16:(t + 1) * 16], self.ident[:], R=["SEL", "ident"], W=[self.PK(bk)], last=(t % 4 == 3))
                if t % 4 == 3:
                    tg = t // 4
                    fw.op("act", lambda tg=tg: nc.scalar.copy(MBT[hb][0:16, tg * 512:(tg + 1) * 512], self.ps[7][0:16, :]), R=[self.PK(7)], W=[("MBT", hb, tg)])
            for lb in range(4):
                q0 = lb * 256
                qv = QT[:, h, q0:q0 + 256]
                items = []
                for n in range(4 * lb + 3):
                    for kh in range(2):
                        kt = n * 2 + kh
                        items.append((KTa[:, kt * 128:(kt + 1) * 128], EN[:, n, :], MBT[hb][:, q0:q0 + 256], Va[:, kt, :], 256, 0,
                                      [wk, ("MBT", hb, lb // 2), "EN"]))
                items.append((KTo[hb][:, q0:q0 + 128], identb, TRI2, Vo[hb][:, 2 * lb, :], 256, 0, [("KTo", hb), ("Vo", hb), "identb", "TRI2"]))
                items.append((KTo[hb][:, q0 + 128:q0 + 256], identb, TRI3, Vo[hb][:, 2 * lb + 1, :], 256, 0, [("KTo", hb), ("Vo", hb), "identb", "TRI3"]))
                ni = len(items)
                for ii, (kl, ml, mr, vt, ncol, c0, keys) in enumerate(items):
                    sb = sk % 2
                    pb = sk % 3
                    sk += 1
                    S = self.ps[sb][:, 0:ncol]
                    fw.mm1(S, kl, qv[:, c0:c0 + ncol], start=True, stop=False, R=keys + [("QT", h)], W=[self.PK(sb)])
                    fw.mm1(S, ml, mr, start=False, stop=True, R=keys, W=[self.PK(sb)], last=True)
                    fw.op("act", lambda S=S, pb=pb, ncol=ncol: nc.scalar.activation(PT[pb][:, 0:ncol], S, AF.Exp), R=[self.PK(sb)], W=[("PT", pb)])
                    fw.mm1(self.ps[2][:, c0:c0 + ncol], vt, PT[pb][:, 0:ncol], start=(ii == 0), stop=(ii == ni - 1), R=keys + [("PT", pb)], W=[self.PK(2)])
                    fw.mm1(self.ps[3][:, c0:c0 + ncol], self.ones_bf[:], PT[pb][:, 0:ncol], start=(ii == 0), stop=(ii == ni - 1), R=["ones", ("PT", pb)], W=[self.PK(3)],
                           last=True)
                rb = lb % 2
                fw.op("dve", lambda rb=rb: nc.vector.reciprocal(RI[rb], self.ps[3][:, 0:256]), R=[self.PK(3)], W=[("RI", rb)])
                fw.op("dve", lambda rb=rb, q0=q0, h=h: nc.vector.tensor_tensor(OT[:, h, q0:q0 + 256], self.ps[2][:, 0:256], RI[rb], op=ALU.mult),
                      R=[self.PK(2), ("RI", rb)], W=[("OT", h)])
        fw.barrier()
        ar.off = mark
        g1bc = ar.f32(D)
        adab = [ar.f32(512), ar.f32(512)]
        tmp = [ar.f32(512), ar.f32(512)]
        self.modsBC(li, 2, g1bc, adab)
        wo3 = self.w_ao[li].rearrange("(k p) f -> p k f", p=128)
        otk = [("OT", h) for h in range(16)]
        k = 0
        for n in range(4):
            def ld(slot, s, n=n):
                fw.dma("pool", slot.rearrange("p (k f) -> p k f", k=KC), wo3[:, :, n * 512:(n + 1) * 512], f"wr{s}", W=[("WR", s)])
            slot, wk = self.ring.next(("wao", li, n), ld)
            w = slot.rearrange("p (k f) -> p k f", k=KC)
            for t in range(NT):
                bk = k % 4
                k += 1
                fw.mm(self.ps[bk][:, :], [(OT[:, hh, t * 128:(t + 1) * 128], w[:, hh, :]) for hh in range(16)], R=[wk] + otk, W=[self.PK(bk)])
                self.resid_evac(bk, t, n, g1bc, tmp[k % 2], ("tmp", k % 2))
        fw.barrier()
        gbc = ar.f32(D)
        bbc = ar.f32(D)
        self.layer_norm(range(NT), self.ln_g[li, 0], self.ln_b[li, 0], gbc, bbc, st, mv)

    def phases(self):
        ph = []
        if self.stage == "A":
            for li in range(2):
                ph.append(lambda li=li: self.gmlp(li))
                ph.append(lambda li=li: self.moe(li))
            ph.append(self.kv)
        else:
            for li in range(2):
                ph.append(lambda li=li: self.attn(li))
                ph.append(lambda li=li: self.moe(li))
        return ph

    def emit(self):
        ph = self.phases()
        if self.stop_after is not None:
            ph = ph[:self.stop_after]
        fw = self.fw
        fw.dry = True
        for p in range(self.npass):
            self.set_pass(p)
            for f in ph:
                f()
        fw.dry = False
        self.ring.reset()
        for p in range(self.npass):
            self.set_pass(p)
            if p > 0:
                fw.barrier()
            self.init_consts(first=(p == 0))
            for f in ph:
                f()
            fw.barrier()
            for t in range(NT):
                self.out_toks.append(fw.dma("sp", self.y_d[t * 128:(t + 1) * 128, :], self.X[:, t, :], f"x{t}", R=[("X", t)]))
        fw.finish(self.out_toks)
        return self.nc


BLOCKS = lambda j: [j, 7 - j, 8 + j, 15 - j]


def _common_maps(inp, layers):
    l0 = layers[0]
    sl = slice(l0, l0 + 2)
    f = lambda a: np.ascontiguousarray(a, dtype=np.float32)
    m = {
        "ada_w": f(inp["ada_w"][sl]), "ada_b": f(inp["ada_b"][sl]),
        "ada_bT": f(inp["ada_b"][sl].reshape(2, 96, 128).transpose(0, 2, 1)),
        "ln_g": f(inp["ln_g"][sl]), "ln_b": f(inp["ln_b"][sl]),
        "w_r": f(inp["moe_w_router"][sl]), "b_rT": f(inp["moe_b_router"][sl].reshape(2, NE, 1)),
        "w_gu": f(inp["moe_w_gu"][sl]),
        "b_guT": f(inp["moe_b_gu"][sl].reshape(2, NE, 8, 128).transpose(0, 3, 1, 2)),
        "w_dn": f(inp["moe_w_down"][sl]), "b_dn": f(inp["moe_b_down"][sl]),
    }
    return m


def _cl(c, b):
    return np.ascontiguousarray(c[b].reshape(KC, 128).T, dtype=np.float32)


_PROG_CACHE = {}


def _get_prog(stage, stop_after=None, npass=1):
    key = (stage, stop_after, npass)
    if key not in _PROG_CACHE:
        p = Prog(stage, stop_after, npass)
        p.emit()
        _PROG_CACHE[key] = p
    return _PROG_CACHE[key]


def _vx(inp, v):
    b, j = v // 4, v % 4
    return np.ascontiguousarray(np.concatenate([inp["x"][b, g * 256:(g + 1) * 256] for g in BLOCKS(j)], axis=0), dtype=np.float32)


def run_stage_a(inp, stop_after=None, groups=None):
    f = lambda a: np.ascontiguousarray(a, dtype=np.float32)
    groups = groups or [[v] for v in range(8)]
    npass = len(groups[0])
    com = _common_maps(inp, [0, 1])
    com.update({
        "w_in": f(inp["gm_w_in"]), "b_in": f(inp["gm_b_in"]),
        "b_inuT": f(inp["gm_b_in"][:, :D].reshape(2, KC, 128).transpose(0, 2, 1)),
        "lnv_gT": f(inp["gm_lnv_g"].reshape(2, KC, 128).transpose(0, 2, 1)),
        "lnv_bT": f(inp["gm_lnv_b"].reshape(2, KC, 128).transpose(0, 2, 1)),
        "w_s": f(inp["gm_w_s"]), "b_s": f(inp["gm_b_s"].reshape(2, 16 * 128)),
        "w_o": f(inp["gm_w_out"]), "kv_ada_w": f(inp["kv_ada_w"]),
        "kv_ada_bT": f(inp["kv_ada_b"].reshape(32, 128).T), "w_kv": f(inp["w_kv"]),
    })
    maps = []
    for grp in groups:
        m = dict(com)
        for p, v in enumerate(grp):
            m[f"x{p}"] = _vx(inp, v)
            m[f"cl{p}"] = _cl(inp["c"], v // 4)
        maps.append(m)
    prog = _get_prog("A", stop_after, npass)
    res = run_bass_kernel_spmd(prog.nc, maps, core_ids=list(range(len(maps)))).results
    ra = {}
    for c, grp in enumerate(groups):
        for p, v in enumerate(grp):
            ra[v] = {k: np.asarray(res[c][f"{k}{p}"]) for k in ("y", "kt_o", "v_o", "km_o")}
    return ra


def run_stage_b(inp, ra, stop_after=None, groups=None):
    f = lambda a: np.ascontiguousarray(a, dtype=np.float32)
    groups = groups or [[v] for v in range(8)]
    npass = len(groups[0])
    com = _common_maps(inp, [2, 3])
    com.update({"w_q": f(inp["attn_w_q"]), "w_ao": f(inp["attn_w_out"])})
    bf = ml_dtypes.bfloat16
    per_batch = {}
    for b in sorted(set(v // 4 for grp in groups for v in grp)):
        kt_all = np.zeros((16, 128, 4096), dtype=bf)
        v_tok = np.zeros((4096, D), dtype=bf)
        km_all = np.zeros((128, 16, 16), dtype=np.float32)
        for j in range(4):
            r = ra[b * 4 + j]
            for lb, g in enumerate(BLOCKS(j)):
                kt_all[:, :, g * 256:(g + 1) * 256] = r["kt_o"][:, :, lb * 256:(lb + 1) * 256]
                v_tok[g * 256:(g + 1) * 256] = r["v_o"][lb * 256:(lb + 1) * 256]
                km_all[:, :, g] = r["km_o"][:, :, lb]
        v_all = np.ascontiguousarray(v_tok.reshape(32, 128, 16, 128).transpose(2, 1, 0, 3))
        per_batch[b] = (kt_all, v_all, km_all)
    maps = []
    for grp in groups:
        m = dict(com)
        for p, v in enumerate(grp):
            b, j = v // 4, v % 4
            r = ra[v]
            m[f"x{p}"] = f(r["y"])
            m[f"cl{p}"] = _cl(inp["c"], b)
            m[f"kt_all{p}"], m[f"v_all{p}"], m[f"km_all{p}"] = per_batch[b]
            m[f"kt_own{p}"] = np.ascontiguousarray(r["kt_o"])
            m[f"v_own{p}"] = np.ascontiguousarray(np.asarray(r["v_o"]).reshape(NT, 128, 16, 128).transpose(2, 1, 0, 3))
            pm = np.zeros((128, NT, 16), dtype=np.float32)
            for lb, g in enumerate(BLOCKS(j)):
                pm[:, 2 * lb:2 * lb + 2, g:] = -1e30
            m[f"pastm{p}"] = pm
        maps.append(m)
    prog = _get_prog("B", stop_after, npass)
    res = run_bass_kernel_spmd(prog.nc, maps, core_ids=list(range(len(maps)))).results
    rb = {}
    for c, grp in enumerate(groups):
        for p, v in enumerate(grp):
            rb[v] = {"y": np.asarray(res[c][f"y{p}"])}
    return rb


def kernel(**inp):
    inp = {k: np.asarray(v) for k, v in inp.items()}
    ra = run_stage_a(inp)
    rb = run_stage_b(inp, ra)
    out = np.zeros((2, 4096, D), dtype=np.float32)
    for v in range(8):
        b, j = v // 4, v % 4
        for lb, g in enumerate(BLOCKS(j)):
            out[b, g * 256:(g + 1) * 256] = rb[v]["y"][lb * 256:(lb + 1) * 256]
    return out
```
